# Optimizing a Trainium2 kernel written in Bass

```python
import math
import jax, jax.numpy as jnp
from jax import lax
import numpy as np

D_MODEL = 4096
BATCH = 8
SEQ = 2048
DEPTH = 1
DEC_BATCH = 16
DEC_SEQ = 32
PAST_LEN = 4096

CHUNK = 64
D_MIX = D_MODEL
D_MLSTM = D_MIX // 2
D_RGLRU = D_MIX - D_MLSTM
MLSTM_HEADS = 4
MLSTM_DV = D_MLSTM // MLSTM_HEADS
MLSTM_DK = MLSTM_DV // 2
RG_BLOCKS = 16
RG_BLOCK_DIM = D_RGLRU // RG_BLOCKS
CONV_WIDTH = 4
RG_C = 8.0
D_FF = -(-8 * D_MODEL // (3 * 256)) * 256
NORM_EPS = 1e-6
PROJ_SIZES = (MLSTM_HEADS * MLSTM_DK, MLSTM_HEADS * MLSTM_DK, D_MLSTM, D_MLSTM,
              MLSTM_HEADS, MLSTM_HEADS, D_RGLRU, D_RGLRU)
D_IN_PROJ = 2 * MLSTM_HEADS * MLSTM_DK + 2 * D_MLSTM + 2 * MLSTM_HEADS + 2 * D_RGLRU

kernel_name = "hybrid_mlstm_rglru_stream_step"


def rms_norm(x, g):
    xf = x.astype(jnp.float32)
    y = xf * lax.rsqrt(jnp.mean(xf * xf, axis=-1, keepdims=True) + NORM_EPS)
    return (y * g.astype(jnp.float32)).astype(x.dtype)


def mlstm_chunkwise(q, k, v, ig, fg, C0, n0, m0):
    B, H, T, DK = q.shape
    L = min(CHUNK, T)
    nc = T // L
    q = q * (DK ** -0.5)
    logf = jax.nn.log_sigmoid(fg)

    def to_chunks(a):
        return jnp.moveaxis(a.reshape(B, H, nc, L, *a.shape[3:]), 2, 0)

    xs = (to_chunks(q), to_chunks(k), to_chunks(v), to_chunks(ig), to_chunks(logf))
    causal = jnp.tril(jnp.ones((L, L), dtype=bool))

    def step(carry, inp):
        C, n, m = carry
        qc, kc, vc, ic, lfc = inp
        b = jnp.cumsum(lfc, axis=-1)
        logD = b[..., :, None] - b[..., None, :] + ic[..., None, :]
        logD = jnp.where(causal, logD, -jnp.inf)
        inter = b + m[..., None]
        m_j = jnp.maximum(inter, jnp.max(logD, axis=-1))
        Dm = jnp.exp(logD - m_j[..., None])
        w_inter = jnp.exp(inter - m_j)
        s = jnp.einsum('bhjd,bhsd->bhjs', qc, kc) * Dm
        num = (w_inter[..., None] * jnp.einsum('bhjd,bhde->bhje', qc, C)
               + jnp.einsum('bhjs,bhse->bhje', s, vc))
        den = w_inter * jnp.einsum('bhjd,bhd->bhj', qc, n) + jnp.sum(s, axis=-1)
        h = num / jnp.maximum(jnp.abs(den), jnp.exp(-m_j))[..., None]
        m_new = m_j[..., -1]
        w_C = jnp.exp(b[..., -1] + m - m_new)
        w_s = jnp.exp(b[..., -1:] - b + ic - m_new[..., None])
        C_new = w_C[..., None, None] * C + jnp.einsum('bhs,bhsd,bhse->bhde', w_s, kc, vc)
        n_new = w_C[..., None] * n + jnp.einsum('bhs,bhsd->bhd', w_s, kc)
        return (C_new, n_new, m_new), h

    (C, n, m), hs = lax.scan(step, (C0, n0, m0), xs)
    h = jnp.moveaxis(hs, 0, 2).reshape(B, H, T, -1)
    return h, C, n, m


def rglru_branch(xb, conv_state, conv_w, conv_b, w_ga, b_ga, w_gx, b_gx, lam, h0, pos0):
    B, T, DR = xb.shape
    xpad = jnp.concatenate([conv_state.astype(jnp.float32), xb.astype(jnp.float32)], axis=1)
    xc = conv_b.astype(jnp.float32)
    for tap in range(CONV_WIDTH):
        xc = xc + xpad[:, tap:tap + T] * conv_w[tap].astype(jnp.float32)
    new_conv = xpad[:, -(CONV_WIDTH - 1):]
    xblk = xc.reshape(B, T, RG_BLOCKS, RG_BLOCK_DIM)
    r = jax.nn.sigmoid(jnp.einsum('btgi,gij->btgj', xblk, w_ga.astype(jnp.float32)).reshape(B, T, DR)
                       + b_ga.astype(jnp.float32))
    i = jax.nn.sigmoid(jnp.einsum('btgi,gij->btgj', xblk, w_gx.astype(jnp.float32)).reshape(B, T, DR)
                       + b_gx.astype(jnp.float32))
    log_a = -RG_C * r * jax.nn.softplus(-lam.astype(jnp.float32))
    a = jnp.exp(log_a)
    mult = jnp.sqrt(-jnp.expm1(2.0 * log_a))
    reset = ((pos0 + jnp.arange(T)) == 0)[None, :, None]
    mult = jnp.where(reset, 1.0, mult)
    a = jnp.where(reset, 0.0, a)
    bterm = mult * (i * xc)
    bterm = bterm.at[:, 0].add(a[:, 0] * h0.astype(jnp.float32))

    def combine(lhs, rhs):
        return (lhs[0] * rhs[0], rhs[0] * lhs[1] + rhs[1])

    _, h = lax.associative_scan(combine, (a, bterm), axis=1)
    return h, h[:, -1], new_conv


def hybrid_layer(x, C0, n0, m0, h0, conv0, pos0, g_pre_mix, w_in, b_igate, b_fgate, g_mlstm_head,
                 conv_w, conv_b, w_rg_a, b_rg_a, w_rg_x, b_rg_x, rg_lambda, w_out, g_post_mix,
                 g_pre_ffn, w_ffn_gate, w_ffn_up, w_ffn_down, g_post_ffn):
    B, T, _ = x.shape
    u = rms_norm(x, g_pre_mix)
    proj = u @ w_in
    split_idx = np.cumsum(PROJ_SIZES)[:-1].tolist()
    q, k, v, o, ig, fg, xr, gr = jnp.split(proj, split_idx, axis=-1)
    f32 = jnp.float32

    def heads(a, d):
        return a.reshape(B, T, MLSTM_HEADS, d).transpose(0, 2, 1, 3).astype(f32)

    ig_h = (ig.astype(f32) + b_igate.astype(f32)).transpose(0, 2, 1)
    fg_h = (fg.astype(f32) + b_fgate.astype(f32)).transpose(0, 2, 1)
    h_m, C1, n1, m1 = mlstm_chunkwise(heads(q, MLSTM_DK), heads(k, MLSTM_DK), heads(v, MLSTM_DV),
                                      ig_h, fg_h, C0, n0, m0)
    h_m = h_m * lax.rsqrt(jnp.mean(h_m * h_m, axis=-1, keepdims=True) + NORM_EPS)
    h_m = h_m * g_mlstm_head.astype(f32).reshape(MLSTM_HEADS, 1, MLSTM_DV)
    h_m = h_m.transpose(0, 2, 1, 3).reshape(B, T, D_MLSTM)
    out_a = jax.nn.sigmoid(o.astype(f32)) * h_m

    h_r, h1, conv1 = rglru_branch(xr, conv0, conv_w, conv_b, w_rg_a, b_rg_a, w_rg_x, b_rg_x,
                                  rg_lambda, h0, pos0)
    out_b = h_r * jax.nn.gelu(gr.astype(f32), approximate=True)

    mix = jnp.concatenate([out_a, out_b], axis=-1).astype(x.dtype) @ w_out
    x = x + rms_norm(mix, g_post_mix)
    u2 = rms_norm(x, g_pre_ffn)
    ffn = (jax.nn.silu(u2 @ w_ffn_gate) * (u2 @ w_ffn_up)) @ w_ffn_down
    x = x + rms_norm(ffn, g_post_ffn)
    return x, C1, n1, m1, h1, conv1


def setup_inputs(seed: int = 0) -> dict:
    key = jax.random.key(seed)
    ks = jax.random.split(key, 32)
    f32 = jnp.float32

    def nrm(k, shape, scale):
        return jax.random.normal(k, shape, f32) * scale

    def gain(k, shape):
        return 1.0 + 0.01 * jax.random.normal(k, shape, f32)

    u = jax.random.uniform(ks[20], (DEPTH, D_RGLRU), f32, 0.9, 0.999)
    s = u ** (1.0 / RG_C)
    rg_lambda = jnp.log(s) - jnp.log1p(-s)
    return {
        "x_prompt": nrm(ks[0], (BATCH, SEQ, D_MODEL), 1.0),
        "x_sample": nrm(ks[1], (DEC_BATCH, DEC_SEQ, D_MODEL), 1.0),
        "state_mlstm_C": nrm(ks[2], (DEPTH, DEC_BATCH, MLSTM_HEADS, MLSTM_DK, MLSTM_DV), 0.1),
        "state_mlstm_n": nrm(ks[3], (DEPTH, DEC_BATCH, MLSTM_HEADS, MLSTM_DK), 0.5),
        "state_mlstm_m": nrm(ks[4], (DEPTH, DEC_BATCH, MLSTM_HEADS), 0.5),
        "state_rglru_h": nrm(ks[5], (DEPTH, DEC_BATCH, D_RGLRU), 0.5),
        "state_rglru_conv": nrm(ks[6], (DEPTH, DEC_BATCH, CONV_WIDTH - 1, D_RGLRU), 1.0),
        "g_pre_mix": gain(ks[7], (DEPTH, D_MODEL)),
        "w_in": nrm(ks[8], (DEPTH, D_MODEL, D_IN_PROJ), D_MODEL ** -0.5),
        "b_igate": nrm(ks[9], (DEPTH, MLSTM_HEADS), 0.1),
        "b_fgate": 3.0 + nrm(ks[10], (DEPTH, MLSTM_HEADS), 0.5),
        "g_mlstm_head": gain(ks[11], (DEPTH, D_MLSTM)),
        "conv_w": nrm(ks[12], (DEPTH, CONV_WIDTH, D_RGLRU), CONV_WIDTH ** -0.5),
        "conv_b": nrm(ks[13], (DEPTH, D_RGLRU), 0.01),
        "w_rg_a": nrm(ks[14], (DEPTH, RG_BLOCKS, RG_BLOCK_DIM, RG_BLOCK_DIM), RG_BLOCK_DIM ** -0.5),
        "b_rg_a": nrm(ks[15], (DEPTH, D_RGLRU), 0.01),
        "w_rg_x": nrm(ks[16], (DEPTH, RG_BLOCKS, RG_BLOCK_DIM, RG_BLOCK_DIM), RG_BLOCK_DIM ** -0.5),
        "b_rg_x": nrm(ks[17], (DEPTH, D_RGLRU), 0.01),
        "rg_lambda": rg_lambda,
        "w_out": nrm(ks[18], (DEPTH, D_MIX, D_MODEL), D_MIX ** -0.5),
        "g_post_mix": gain(ks[19], (DEPTH, D_MODEL)),
        "g_pre_ffn": gain(ks[21], (DEPTH, D_MODEL)),
        "w_ffn_gate": nrm(ks[22], (DEPTH, D_MODEL, D_FF), D_MODEL ** -0.5),
        "w_ffn_up": nrm(ks[23], (DEPTH, D_MODEL, D_FF), D_MODEL ** -0.5),
        "w_ffn_down": nrm(ks[24], (DEPTH, D_FF, D_MODEL), D_FF ** -0.5),
        "g_post_ffn": gain(ks[25], (DEPTH, D_MODEL)),
    }


def reference(x_prompt, x_sample, state_mlstm_C, state_mlstm_n, state_mlstm_m, state_rglru_h,
              state_rglru_conv, g_pre_mix, w_in, b_igate, b_fgate, g_mlstm_head, conv_w, conv_b,
              w_rg_a, b_rg_a, w_rg_x, b_rg_x, rg_lambda, w_out, g_post_mix, g_pre_ffn,
              w_ffn_gate, w_ffn_up, w_ffn_down, g_post_ffn):
    f32 = jnp.float32
    B = x_prompt.shape[0]
    yp, ys = x_prompt, x_sample
    pC, pn, pm, ph, pconv = [], [], [], [], []
    sC, sn, sm, sh, sconv = [], [], [], [], []
    for l in range(DEPTH):
        w = (g_pre_mix[l], w_in[l], b_igate[l], b_fgate[l], g_mlstm_head[l], conv_w[l], conv_b[l],
             w_rg_a[l], b_rg_a[l], w_rg_x[l], b_rg_x[l], rg_lambda[l], w_out[l], g_post_mix[l],
             g_pre_ffn[l], w_ffn_gate[l], w_ffn_up[l], w_ffn_down[l], g_post_ffn[l])
        C0 = jnp.zeros((B, MLSTM_HEADS, MLSTM_DK, MLSTM_DV), f32)
        n0 = jnp.zeros((B, MLSTM_HEADS, MLSTM_DK), f32)
        m0 = jnp.zeros((B, MLSTM_HEADS), f32)
        h0 = jnp.zeros((B, D_RGLRU), f32)
        cv0 = jnp.zeros((B, CONV_WIDTH - 1, D_RGLRU), f32)
        yp, c1, n1, m1, h1, cv1 = hybrid_layer(yp, C0, n0, m0, h0, cv0, 0, *w)
        pC.append(c1); pn.append(n1); pm.append(m1); ph.append(h1); pconv.append(cv1)
        ys, c2, n2, m2, h2, cv2 = hybrid_layer(
            ys, state_mlstm_C[l].astype(f32), state_mlstm_n[l].astype(f32), state_mlstm_m[l].astype(f32),
            state_rglru_h[l], state_rglru_conv[l], PAST_LEN, *w)
        sC.append(c2); sn.append(n2); sm.append(m2); sh.append(h2); sconv.append(cv2)
    p_C, p_n, p_m, p_h, p_conv = jnp.stack(pC), jnp.stack(pn), jnp.stack(pm), jnp.stack(ph), jnp.stack(pconv)
    s_C, s_n, s_m, s_h, s_conv = jnp.stack(sC), jnp.stack(sn), jnp.stack(sm), jnp.stack(sh), jnp.stack(sconv)
    return (yp, ys, p_C, p_n, p_m, p_h, p_conv, s_C, s_n, s_m, s_h, s_conv)
```

```python
import contextlib
import os
import numpy as np
import ml_dtypes
import concourse.bass as bass
import concourse.mybir as mybir
from concourse.bass_utils import run_bass_kernel_spmd

F32 = mybir.dt.float32
BF16 = mybir.dt.bfloat16
AF = mybir.ActivationFunctionType
ALU = mybir.AluOpType
AX = mybir.AxisListType

D = 4096
NCH = 32
DFF = 11008
DIN = 10248
NT = 256
BIG = 30000.0
EPS = 1e-6
NWBUF = 3
COMPUTE = ("pe", "act", "dve", "pool")
MK_STAGE = float(os.environ.get("MK_STAGE", "99"))


class StopBuild(Exception):
    pass


def stage(n):
    if MK_STAGE < n:
        raise StopBuild()


class Sched:
    def __init__(self):
        self.ops = {e: [] for e in ("pe", "act", "dve", "pool", "sp")}
        self.cnt = {}
        self.epoch = 0
        self.waited = {e: {} for e in self.ops}
        self.lastw = {}
        self.readers = {}
        self.dma_pool = {"sp": [("dsp", i) for i in range(8)], "pool": [("dpl", i) for i in range(6)]}
        self.dma_rr = {"sp": 0, "pool": 0}
        self.dma_last = {}
        self.semkeys = set()

    def cur(self, eng):
        return (eng, self.epoch)

    def _deps(self, reads, writes):
        deps = []
        for k in reads:
            t = self.lastw.get(k)
            if t is not None:
                deps.append(t)
        for k in writes:
            t = self.lastw.get(k)
            if t is not None:
                deps.append(t)
            r = self.readers.get(k)
            if r:
                deps.extend(r.items())
        return deps

    def _filter(self, eng, deps):
        best = {}
        w = self.waited[eng]
        for sk, v in deps:
            if eng == "pe" and sk[0] == "pe":
                continue
            if w.get(sk, 0) >= v:
                continue
            if best.get(sk, 0) < v:
                best[sk] = v
        for sk, v in best.items():
            w[sk] = v
        return list(best.items())

    def _register(self, tok, reads, writes):
        sk, v = tok
        for k in reads:
            r = self.readers.setdefault(k, {})
            if r.get(sk, 0) < v:
                r[sk] = v
        for k in writes:
            self.lastw[k] = tok
            self.readers[k] = {}

    def op(self, eng, fn, reads=(), writes=(), signal=True):
        assert signal or eng == "pe"
        psr = [k for k in reads if isinstance(k, tuple) and k[0] == "ps"]
        if psr:
            reads = [k for k in reads if k not in psr]
            writes = list(writes) + psr
        waits = self._filter(eng, self._deps(reads, writes))
        sk = self.cur(eng)
        self.semkeys.add(sk)
        val = self.cnt.get(sk, 0) + 1
        if signal:
            self.cnt[sk] = val
        self._register((sk, val), reads, writes)
        self.ops[eng].append((waits, fn, (sk, 1) if signal else None))

    def dma(self, q, fn, reads=(), writes=()):
        deps = self._deps(reads, writes)
        pool = self.dma_pool[q]
        sk = pool[self.dma_rr[q] % len(pool)]
        self.dma_rr[q] += 1
        self.semkeys.add(sk)
        prev = self.dma_last.get(sk)
        if prev:
            deps.append((sk, prev))
        waits = self._filter(q, deps)
        val = self.cnt.get(sk, 0) + 16
        self.cnt[sk] = val
        self.dma_last[sk] = val
        self._register((sk, val), reads, writes)
        self.ops[q].append((waits, fn, (sk, 16)))

    def finish(self):
        waits = self._filter("sp", [(sk, v) for sk, v in self.dma_last.items()])
        self.ops["sp"].append((waits, None, None))


def build_nc(n_ptiles):
    SEQ = n_ptiles * NT
    nc = bass.Bass("TRN2", target_bir_lowering=False)
    S = Sched()

    def din(name, shape, dt=F32):
        return nc.dram_tensor(name, list(shape), dt, kind="ExternalInput").ap()

    def dout(name, shape, dt=F32):
        return nc.dram_tensor(name, list(shape), dt, kind="ExternalOutput").ap()

    xp = din("xp", [SEQ, D]); xs = din("xs", [64, D])
    sC_in = din("sC", [2, 4, 256, 512]); sn_in = din("sn", [128, 2, 4, 2]); sm_in = din("sm", [128, 2, 4])
    sh_in = din("sh", [128, 2, 16]); sconv_in = din("sconv", [128, 2, 16, 3])
    w_in = din("w_in", [D, DIN]); w_out = din("w_out", [D, D])
    w_g = din("w_g", [D, DFF]); w_u = din("w_u", [D, DFF]); w_d = din("w_d", [DFF, D])
    w_rga = din("w_rga", [16, 128, 128]); w_rgx = din("w_rgx", [16, 128, 128])
    vecs_d = din("vecs", [128, 352])
    bif_d = din("bif", [128, 8])
    cf_d = din("cf", [128, 5 * 128 + 8])
    cb_d = din("cb", [128, 256], BF16)

    yp = dout("yp", [SEQ, D]); ys = dout("ys", [64, D])
    pC = dout("pC", [4, 256, 512]); pn = dout("pn", [128, 4, 2]); pm = dout("pm", [128, 4])
    ph = dout("ph", [128, 16]); pconv = dout("pconv", [128, 16, 3])
    oC = dout("oC", [2, 4, 256, 512]); on = dout("on", [128, 2, 4, 2]); om = dout("om", [128, 2, 4])
    oh = dout("oh", [128, 2, 16]); oconv = dout("oconv", [128, 2, 16, 3])

    SCR_PER = 64
    scr = [nc.dram_tensor(f"wscr{i}", [SCR_PER, 128, 4096], BF16, kind="Internal").ap() for i in range(6)]

    es = contextlib.ExitStack()

    def sb(name, shape, dt=F32):
        return es.enter_context(nc.sbuf_tensor(name, list(shape), dt))

    with es:
        A = sb("A", [128, NCH, NT]); B = sb("B", [128, NCH, NT])
        U = sb("U", [128, NCH, NT], BF16); Dm = sb("Dm", [128, NCH, NT], BF16)
        W = [sb(f"W{i}", [128, 4096], BF16) for i in range(NWBUF)]
        Cst = sb("Cst", [128, 4, 2, 512]); nst = sb("nst", [128, 4, 2]); mst = sb("mst", [128, 4])
        hst = sb("hst", [128, 3, 16]); cvst = sb("cvst", [128, 3, 16, 3])
        wif = sb("wif", [128, 32, 8], BF16)
        wga = sb("wga", [128, 16, 128], BF16); wgx = sb("wgx", [128, 16, 128], BF16)
        vecs = sb("vecs_s", [128, 352]); bif = sb("bif_s", [128, 8])
        cf = sb("cf_s", [128, 5 * 128 + 8]); cb = sb("cb_s", [128, 256], BF16)
        nsp = sb("nsp", [128, 32])
        stg = [sb(f"stg{i}", [128, 1024]) for i in range(2)]
        acc = sb("acc", [128, NT]); sqt = [sb(f"sqt{i}", [128, NT]) for i in range(2)]
        rstd = sb("rstd", [128, NT]); lnv = sb("lnv", [128, NT])
        qT = sb("qT", [128, 2, 2, NT], BF16); kT = sb("kT", [128, 2, 2, NT], BF16)
        ktok = sb("ktok", [128, 2, 2, 256], BF16); vtok = sb("vtok", [128, 2, 2, 512], BF16)
        sig = sb("sig", [128, 2, 2, 512], BF16)
        igf = sb("igf", [128, 2, 8]); sp8 = sb("sp8", [128, 2, 8]); cvec = sb("cvec", [128, 2, 4])
        nb = sb("nb", [128, 2, 4]); nbl = sb("nbl", [128, 2, 4])
        dg = sb("dg", [128, 128]); dg2 = sb("dg2", [128, 128]); tll = sb("tll", [128, 128])
        dmT = sb("dmT", [128, 128]); PT = sb("PT", [128, 128], BF16)
        t1 = sb("t1", [128, 512]); t2 = sb("t2", [128, 512]); t4 = sb("t4", [128, 512], BF16)
        kw = sb("kw", [128, 256], BF16); Cbf = sb("Cbf", [128, 2, 512], BF16); nbf = sb("nbf", [128, 2], BF16)
        sv = sb("sv", [128, 24])
        xrh = sb("xrh", [128, 2, 2, NT + 8]); grb = sb("grb", [128, 2, 2, NT])
        xc = sb("xc", [128, NT]); xcb = sb("xcb", [128, NT], BF16)
        rg_r = sb("rg_r", [128, NT]); rg_i = sb("rg_i", [128, NT]); rg_a = sb("rg_a", [128, NT])
        rg_m = sb("rg_m", [128, NT]); rg_b = sb("rg_b", [128, NT]); rg_h = sb("rg_h", [128, NT])
        rg_t = sb("rg_t", [128, NT])
        PS = [es.enter_context(nc.psum_tensor(f"ps{i}", [128, 512], F32)) for i in range(8)]

        ident = cf[:, 0:128]; tri = cf[:, 128:256]; maskA = cf[:, 256:384]; maskB = cf[:, 384:512]
        ones = cf[:, 512:640]; cst = cf[:, 640:648]
        eps_ap = cst[:, 0:1]; one_ap = cst[:, 1:2]
        identb = cb[:, 0:128]; onesb = cb[:, 128:256]
        gpre = vecs[:, 0:32]; gpm = vecs[:, 32:64]; gpf = vecs[:, 64:96]; gpo = vecs[:, 96:128]
        ghead = vecs[:, 128:144]; cw = vecs[:, 144:208]; convb = vecs[:, 208:224]
        bga = vecs[:, 224:240]; bgx = vecs[:, 240:256]; lam = vecs[:, 256:272]

        free_banks = list(range(8))

        def bank():
            assert free_banks, "PSUM banks exhausted"
            return free_banks.pop(0)

        def free(*bs):
            for b in bs:
                assert b not in free_banks
                free_banks.append(b)

        def pk(i):
            return ("ps", i)

        def act(out, in_, func, bias=None, scale=None, accum=None, r=(), w=()):
            def fn(e):
                kw_ = {}
                if bias is not None:
                    kw_["bias"] = bias
                if scale is not None:
                    kw_["scale"] = scale
                if accum is not None:
                    kw_["accum_out"] = accum
                return e.activation(out=out, in_=in_, func=func, **kw_)
            S.op("act", fn, r, w)

        def tt(out, in0, in1, op, r=(), w=(), eng="dve"):
            S.op(eng, lambda e: e.tensor_tensor(out=out, in0=in0, in1=in1, op=op), r, w)

        def ts(out, in0, s1, op0, s2=None, op1=None, r=(), w=(), eng="dve"):
            def fn(e):
                if op1 is None:
                    return e.tensor_scalar(out=out, in0=in0, scalar1=s1, scalar2=None, op0=op0)
                return e.tensor_scalar(out=out, in0=in0, scalar1=s1, scalar2=s2, op0=op0, op1=op1)
            S.op(eng, fn, r, w)

        def stt(out, in0, sc, in1, op0, op1, r=(), w=()):
            S.op("dve", lambda e: e.scalar_tensor_tensor(out=out, in0=in0, scalar=sc, in1=in1, op0=op0, op1=op1), r, w)

        def cp(out, in_, r=(), w=(), eng="dve"):
            S.op(eng, lambda e: e.tensor_copy(out=out, in_=in_), r, w)

        def mm(out, lhsT, rhs, start, stop, r=(), w=(), signal=None):
            S.op("pe", lambda e: e.matmul(out, lhsT=lhsT, rhs=rhs, start=start, stop=stop), r, w,
                 signal=stop if signal is None else signal)

        def tr(out, in_, idn, r=(), w=(), signal=True):
            S.op("pe", lambda e: e.transpose(out=out, in_=in_, identity=idn), r, w, signal=signal)

        def dma(q, out, in_, r=(), w=(), nonc=False):
            def fn(e):
                with nc.allow_non_contiguous_dma(reason="small"):
                    return e.dma_start(out=out, in_=in_)
            S.dma(q, fn, r, w)

        evrr = [0]

        def evac_copy(out, in_, r, w):
            evrr[0] += 1
            if evrr[0] % 2:
                cp(out, in_, r, w)
            else:
                act(out, in_, AF.Copy, r=r, w=w)

        lanes = {"a": [], "b": []}

        def tick1(q):
            while q:
                try:
                    next(q[0])
                    return
                except StopIteration:
                    q.pop(0)

        def tick():
            tick1(lanes["a"])
            tick1(lanes["b"])

        def drain(lane=None, maxlen=0):
            for nm in ([lane] if lane else ["a", "b"]):
                q = lanes[nm]
                while len(q) > maxlen:
                    tick1(q)
                    if lane is None or True:
                        other = lanes["b" if nm == "a" else "a"]
                        tick1(other)

        wrr = [0]

        slab_idx = [0]
        slab_mode = ["cast"]

        def slab(wd, r0, nk, c0, ncol):
            i = wrr[0] % NWBUF
            wrr[0] += 1
            si = slab_idx[0]
            slab_idx[0] += 1
            flat = W[i][:, 0:nk * ncol]
            view = flat.rearrange("p (k c) -> p k c", k=nk)
            sdst = scr[si // SCR_PER][si % SCR_PER][:, 0:nk * ncol]
            if slab_mode[0] == "cast":
                src = wd[r0:r0 + nk * 128, c0:c0 + ncol].rearrange("(k p) c -> p k c", p=128)
                dma("pool", view, src, w=[("W", i)])
                dma("sp", sdst, flat, r=[("W", i)], w=[("scr", si)])
            else:
                dma("pool", flat, sdst, r=[("scr", si)], w=[("W", i)])
            return view, ("W", i)

        def ws_group(wd, c0, nch, K, rhs_of, rkeys_of, ncols, consume, hold=False):
            ncol = nch * 128
            nk_max = 4096 // ncol
            banks = [bank() for _ in range(nch)]
            k0 = 0
            while k0 < K:
                nk = min(nk_max, K - k0)
                view, wkey = slab(wd, k0 * 128, nk, c0, ncol)
                for j in range(nch):
                    for kk in range(nk):
                        k = k0 + kk
                        last_in_slab = (j == nch - 1 and kk == nk - 1)
                        mm(PS[banks[j]][:, 0:ncols], view[:, kk, j * 128:(j + 1) * 128], rhs_of(k),
                           start=(k == 0), stop=(k == K - 1), r=[wkey] + rkeys_of(k), w=[pk(banks[j])],
                           signal=(k == K - 1) or last_in_slab)
                k0 += nk
                tick()
            for j in range(nch):
                consume(j, banks[j])
                if not hold:
                    free(banks[j])

        def as_group(wd, c0, blocks, consume):
            banks = [bank() for _ in blocks]
            for k0 in range(0, 32, 8):
                view, wkey = slab(wd, k0 * 128, 8, c0, 512)
                for bi, (b0, L) in enumerate(blocks):
                    for kk in range(8):
                        k = k0 + kk
                        last_in_slab = (bi == len(blocks) - 1 and kk == 7)
                        mm(PS[banks[bi]][0:L, :], U[:, k, b0:b0 + L], view[:, kk, :],
                           start=(k == 0), stop=(k == 31), r=[wkey, ("U", k)], w=[pk(banks[bi])],
                           signal=(k == 31) or last_in_slab)
                tick()
            for bi in range(len(blocks)):
                consume(bi, banks[bi])
                free(banks[bi])

        def finish_rstd(ntok, dim):
            bi = bank()
            mm(PS[bi][:, 0:ntok], ones, acc[:, 0:ntok], True, True, r=["acc"], w=[pk(bi)])
            act(lnv[:, 0:ntok], PS[bi][:, 0:ntok], AF.Ln, bias=eps_ap, scale=1.0 / dim, r=[pk(bi)], w=["lnv"])
            act(rstd[:, 0:ntok], lnv[:, 0:ntok], AF.Exp, scale=-0.5, r=["lnv"], w=["rstd"])
            free(bi)

        def accum_sq(c, src, srckeys, ntok):
            if c == 0:
                act(acc[:, 0:ntok], src, AF.Square, r=srckeys, w=["acc"])
            else:
                s = sqt[c % 2]
                act(s[:, 0:ntok], src, AF.Square, r=srckeys, w=[("sqt", c % 2)])
                tt(acc[:, 0:ntok], acc[:, 0:ntok], s[:, 0:ntok], ALU.add, r=[("sqt", c % 2), "acc"], w=["acc"])

        dma("sp", cf[:], cf_d, w=["cf"]); dma("sp", cb[:], cb_d, w=["cb"])
        dma("sp", vecs[:], vecs_d, w=["vecs"]); dma("sp", bif[:], bif_d, w=["bif"])
        dma("pool", wif[:], w_in[:, 6144:6152].rearrange("(k p) c -> p k c", p=128), w=["wif"], nonc=True)
        dma("pool", wga[:], w_rga.rearrange("g i j -> i g j"), w=["wga"])
        dma("pool", wgx[:], w_rgx.rearrange("g i j -> i g j"), w=["wgx"])
        act(nsp[:, 0:16], lam, AF.Exp, scale=-1.0, r=["vecs"], w=["nsp"])
        act(nsp[:, 16:32], nsp[:, 0:16], AF.Ln, bias=one_ap, r=["nsp", "cf"], w=["nsp"])
        ts(nsp[:, 0:16], nsp[:, 16:32], -8.0, ALU.mult, r=["nsp"], w=["nsp"])
        ts(nsp[:, 16:32], nsp[:, 16:32], -16.0, ALU.mult, r=["nsp"], w=["nsp"])
        S.op("dve", lambda e: e.memset(hst[:, 0, :], 0.0), (), ["hst"])
        S.op("dve", lambda e: e.memset(cvst[:, 0].rearrange("p a b -> p (a b)"), 0.0), (), ["cvst"])
        dma("sp", hst[:, 1:3, :], sh_in, w=["hst"])
        dma("sp", cvst[:, 1:3], sconv_in, w=["cvst"])
        const_keys = ["cf", "cb", "vecs", "bif", "wif", "wga", "wgx", "nsp"]

        def do_tile(ti, kind):
            S.epoch = ti + 1 if kind == "p" else 0
            slab_idx[0] = 0
            slab_mode[0] = "cast" if kind == "s" else "scr"
            if kind == "p" and ti == 0:
                S.op("dve", lambda e: e.memset(Cst[:].rearrange("p a b c -> p (a b c)"), 0.0), (), [("Cst", h) for h in range(4)])
                S.op("dve", lambda e: e.memset(nst[:].rearrange("p a b -> p (a b)"), 0.0), (), ["nst"])
                S.op("dve", lambda e: e.memset(mst[:], 0.0), (), ["mst"])
            if kind == "p":
                xd, yd, row0 = xp, yp, ti * NT
                blocks = [(0, 128), (128, 128)]
                nseg, Lseg, ntok = 1, NT, NT
                slots = [0]
            else:
                xd, yd, row0 = xs, ys, 0
                blocks = [(0, 32), (32, 32)]
                nseg, Lseg, ntok = 2, 32, 64
                slots = [1, 2]
            first_prompt = (kind == "p" and ti == 0)
            last_prompt = (kind == "p" and ti == n_ptiles - 1)
            Ak = lambda c: ("A", c)
            Bk = lambda c: ("B", c)
            Uk = lambda c: ("U", c)
            Dk = lambda c: ("D", c)

            for bi, (b0, L) in enumerate(blocks):
                for pc in range(4):
                    st = stg[pc % 2]; sk = ("stg", pc % 2)
                    dma("sp", st[0:L, :], xd[row0 + b0:row0 + b0 + L, pc * 1024:(pc + 1) * 1024], w=[sk])
                    for half in range(2):
                        bk = bank()
                        for j in range(4):
                            tr(PS[bk][:, j * L:(j + 1) * L], st[0:L, (half * 4 + j) * 128:(half * 4 + j + 1) * 128],
                               ident[0:L, 0:L], r=[sk, "cf"], w=[pk(bk)], signal=(j == 3))
                        c0 = pc * 8 + half * 4
                        evac_copy(A[:, c0:c0 + 4, b0:b0 + L], PS[bk][:, 0:4 * L].rearrange("p (j l) -> p j l", j=4),
                                  r=[pk(bk)], w=[Ak(c0 + j) for j in range(4)])
                        free(bk)
            stage(1)
            for c in range(NCH):
                accum_sq(c, A[:, c, 0:ntok], [Ak(c)], ntok)
            finish_rstd(ntok, D)
            for c in range(NCH):
                stt(U[:, c, 0:ntok], A[:, c, 0:ntok], gpre[:, c:c + 1], rstd[:, 0:ntok], ALU.mult, ALU.mult,
                    r=[Ak(c), "rstd", "vecs"], w=[Uk(c)])

            u_rhs = lambda k: U[:, k, 0:ntok]
            u_keys = lambda k: [Uk(k)]

            stage(2)
            for bi, (b0, L) in enumerate(blocks):
                bk = bank()
                for k in range(32):
                    mm(PS[bk][0:L, 0:8], U[:, k, b0:b0 + L], wif[:, k, :], k == 0, k == 31,
                       r=[Uk(k), "wif"], w=[pk(bk)])
                tt(igf[0:L, bi, :], PS[bk][0:L, 0:8], bif[0:L, :], ALU.add, r=[pk(bk), "bif"], w=[("igf", bi)])
                free(bk)
                act(sp8[0:L, bi, 0:4], igf[0:L, bi, 4:8], AF.Exp, scale=-1.0, r=[("igf", bi)], w=[("sp8", bi)])
                act(sp8[0:L, bi, 4:8], sp8[0:L, bi, 0:4], AF.Ln, bias=one_ap[0:L], r=[("sp8", bi), "cf"], w=[("sp8", bi)])
                bk2 = bank()
                mm(PS[bk2][0:L, 0:4], tri[0:L, 0:L], sp8[0:L, bi, 4:8], True, True, r=[("sp8", bi), "cf"], w=[pk(bk2)])
                mm(PS[bk2][:, 4:8], ones[0:L, :], sp8[0:L, bi, 4:8], True, True, r=[("sp8", bi), "cf"], w=[pk(bk2)])
                cp(nb[0:L, bi, :], PS[bk2][0:L, 0:4], r=[pk(bk2)], w=[("nb", bi)])
                cp(nbl[:, bi, :], PS[bk2][:, 4:8], r=[pk(bk2)], w=[("nbl", bi)])
                free(bk2)
                tt(cvec[0:L, bi, :], igf[0:L, bi, 0:4], nb[0:L, bi, :], ALU.add, r=[("igf", bi), ("nb", bi)], w=[("cvec", bi)])

            stage(3)
            def chunk_gen(h, hb):
                for bi, (b0, L) in enumerate(blocks):
                    if kind == "p":
                        slot = h
                    else:
                        slot = (2 * h + bi) % 4
                        dma("sp", Cst[:, slot], sC_in[bi, h].rearrange("(j p) e -> p j e", p=128), w=[("Cst", slot)])
                        dma("sp", nst[:, slot, :], sn_in[:, bi, h, :], w=["nst"])
                        dma("sp", mst[:, slot:slot + 1], sm_in[:, bi, h:h + 1], w=["mst"])
                    Ck = ("Cst", slot)
                    qk = lambda j: ("qT", hb, j)
                    kk_ = lambda j: ("kT", hb, j)
                    vk = ("vtok", hb, bi); sgk = ("sig", hb, bi); ktk = ("ktok", hb, bi)
                    c_h = cvec[0:L, bi, h:h + 1]
                    m_b = mst[:, slot:slot + 1]
                    cm = sv[0:L, 0:1]; g = sv[0:L, 1:2]; glb = sv[:, 2:3]; nglb = sv[:, 3:4]
                    winter = sv[0:L, 4:5]; r2 = sv[0:L, 5:7]; den = sv[0:L, 7:8]; mj = sv[0:L, 8:9]
                    emj = sv[0:L, 9:10]; rden = sv[0:L, 10:11]; ss = sv[0:L, 11:12]; vv = sv[0:L, 12:13]
                    sc = sv[0:L, 13:14]; wsv = sv[0:L, 14:15]; wC = sv[:, 15:16]
                    bs = bank()
                    for j in range(2):
                        mm(PS[bs][0:L, 0:L], kT[:, hb, j, b0:b0 + L], qT[:, hb, j, b0:b0 + L], j == 0, j == 1,
                           r=[kk_(j), qk(j)], w=[pk(bs)])
                    cp(Cbf[:].rearrange("p a b -> p (a b)"), Cst[:, slot].rearrange("p a b -> p (a b)"), r=[Ck], w=["Cbf"])
                    cp(nbf[:], nst[:, slot, :], r=["nst"], w=["nbf"])
                    ts(dg[0:L, 0:L], ident[0:L, 0:L], c_h, ALU.mult, r=[("cvec", bi), "cf"], w=["dg"])
                    yield
                    bx = bank()
                    mm(PS[bx][0:L, 0:L], ones[0:L, 0:L], dg[0:L, 0:L], True, True, r=["dg", "cf"], w=[pk(bx)])
                    tt(tll[0:L, 0:L], PS[bx][0:L, 0:L], maskA[0:L, 0:L], ALU.add, r=[pk(bx), "cf"], w=["tll"])
                    free(bx)
                    S.op("dve", lambda e, cm=cm, L=L: e.reduce_max(out=cm, in_=tll[0:L, 0:L], axis=AX.X), ["tll"], ["sv"])
                    tt(g, cm, m_b[0:L], ALU.max, r=["sv", "mst"], w=["sv"])
                    ts(dg2[0:L, 0:L], ident[0:L, 0:L], g, ALU.mult, r=["sv", "cf"], w=["dg2"])
                    yield
                    by = bank()
                    mm(PS[by][:, 0:L], ones[0:L, :], dg2[0:L, 0:L], True, True, r=["dg2", "cf"], w=[pk(by)])
                    tt(tll[0:L, 0:L], PS[by][0:L, 0:L], maskB[0:L, 0:L], ALU.add, r=[pk(by), "cf"], w=["tll"])
                    cp(glb, PS[by][:, L - 1:L], r=[pk(by)], w=["sv"])
                    free(by)
                    act(dmT[0:L, 0:L], tll[0:L, 0:L], AF.Exp, bias=c_h, scale=-1.0, r=["tll", ("cvec", bi)], w=["dmT"])
                    ts(nglb, glb, -1.0, ALU.mult, r=["sv"], w=["sv"])
                    tt(PT[0:L, 0:L], PS[bs][0:L, 0:L], dmT[0:L, 0:L], ALU.mult, r=[pk(bs), "dmT"], w=["PT"])
                    free(bs)
                    act(winter, g, AF.Exp, bias=m_b[0:L], scale=-1.0, r=["sv", "mst"], w=["sv"])
                    tt(mj, g, nb[0:L, bi, h:h + 1], ALU.subtract, r=["sv", ("nb", bi)], w=["sv"])
                    act(emj, mj, AF.Exp, scale=-1.0, r=["sv"], w=["sv"])
                    act(wsv, c_h, AF.Exp, bias=nglb[0:L], r=[("cvec", bi), "sv"], w=["sv"])
                    act(wC, m_b, AF.Exp, bias=nglb, r=["mst", "sv"], w=["sv"])
                    ts(kw[0:L, :], ktok[0:L, hb, bi, :], wsv, ALU.mult, r=[ktk, "sv"], w=["kw"])
                    yield
                    bn = bank()
                    mm(PS[bn][0:L, :], PT[0:L, 0:L], vtok[0:L, hb, bi, :], True, True, r=["PT", vk], w=[pk(bn)])
                    bi_ = bank()
                    for j in range(2):
                        mm(PS[bi_][0:L, :], qT[:, hb, j, b0:b0 + L], Cbf[:, j, :], j == 0, j == 1,
                           r=[qk(j), "Cbf"], w=[pk(bi_)])
                    br = bank()
                    mm(PS[br][0:L, 0:1], PT[0:L, 0:L], onesb[0:L, 0:1], True, True, r=["PT", "cb"], w=[pk(br)])
                    for j in range(2):
                        mm(PS[br][0:L, 1:2], qT[:, hb, j, b0:b0 + L], nbf[:, j:j + 1], j == 0, j == 1,
                           r=[qk(j), "nbf"], w=[pk(br)])
                    bc = [bank(), bank()]
                    for j in range(2):
                        mm(PS[bc[j]][:, :], kw[0:L, j * 128:(j + 1) * 128], vtok[0:L, hb, bi, :], True, True,
                           r=["kw", vk], w=[pk(bc[j])])
                    bd = br
                    for j in range(2):
                        mm(PS[bd][:, 2 + j:3 + j], kw[0:L, j * 128:(j + 1) * 128], onesb[0:L, 0:1], True, True,
                           r=["kw", "cb"], w=[pk(bd)])
                    cp(r2, PS[br][0:L, 0:2], r=[pk(br)], w=["sv"])
                    stt(den, r2[:, 1:2], winter, r2[:, 0:1], ALU.mult, ALU.add, r=["sv"], w=["sv"])
                    stt(den, den, -1.0, den, ALU.mult, ALU.max, r=["sv"], w=["sv"])
                    tt(den, den, emj, ALU.max, r=["sv"], w=["sv"])
                    S.op("dve", lambda e, rden=rden, den=den: e.reciprocal(out=rden, in_=den), ["sv"], ["sv"])
                    act(t1[0:L, :], PS[bi_][0:L, :], AF.Copy, scale=winter, r=[pk(bi_), "sv"], w=["t1"])
                    tt(t2[0:L, :], PS[bn][0:L, :], t1[0:L, :], ALU.add, r=[pk(bn), "t1"], w=["t2"])
                    for j in range(2):
                        stt(Cst[:, slot, j, :], Cst[:, slot, j, :], wC, PS[bc[j]][:, :], ALU.mult, ALU.add,
                            r=[Ck, "sv", pk(bc[j])], w=[Ck])
                    stt(nst[:, slot, :], nst[:, slot, :], wC, PS[bd][:, 2:4], ALU.mult, ALU.add,
                        r=["nst", "sv", pk(bd)], w=["nst"])
                    free(bn, bi_, br, bc[0], bc[1])
                    tt(m_b, glb, nbl[:, bi, h:h + 1], ALU.subtract, r=["sv", ("nbl", bi)], w=["mst"])
                    act(t1[0:L, :], t2[0:L, :], AF.Square, accum=ss, r=["t2"], w=["t1", "sv"])
                    tt(vv, rden, rden, ALU.mult, r=["sv"], w=["sv"])
                    tt(vv, vv, ss, ALU.mult, r=["sv"], w=["sv"])
                    act(vv, vv, AF.Ln, bias=eps_ap[0:L], scale=1.0 / 512.0, r=["sv", "cf"], w=["sv"])
                    act(vv, vv, AF.Exp, scale=-0.5, r=["sv"], w=["sv"])
                    tt(sc, vv, rden, ALU.mult, r=["sv"], w=["sv"])
                    stt(t4[0:L, :], t2[0:L, :], sc, sig[0:L, hb, bi, :], ALU.mult, ALU.mult, r=["t2", "sv", sgk], w=["t4"])
                    if kind == "s":
                        dma("sp", oC[bi, h].rearrange("(j p) e -> p j e", p=128), Cst[:, slot], r=[Ck])
                        dma("sp", on[:, bi, h, :], nst[:, slot, :], r=["nst"])
                        dma("sp", om[:, bi, h:h + 1], mst[:, slot:slot + 1], r=["mst"])
                    elif last_prompt and bi == len(blocks) - 1:
                        dma("sp", pC[h].rearrange("(j p) e -> p j e", p=128), Cst[:, slot], r=[Ck])
                    yield
                    bt_ = bank()
                    pb = PS[bt_][:].bitcast(BF16)
                    for ec in range(4):
                        tr(pb[:, ec * L:(ec + 1) * L], t4[0:L, ec * 128:(ec + 1) * 128], identb[0:L, 0:L],
                           r=["t4", "cb"], w=[pk(bt_)], signal=(ec == 3))
                    for ec in range(4):
                        c = h * 4 + ec
                        ts(Dm[:, c, b0:b0 + L], pb[:, ec * L:(ec + 1) * L], ghead[:, c:c + 1], ALU.mult,
                           r=[pk(bt_), "vecs"], w=[Dk(c)])
                    free(bt_)
                    yield
                if last_prompt and h == 3:
                    dma("sp", pn, nst[:], r=["nst"])
                    dma("sp", pm, mst[:], r=["mst"])

            def do_head(h):
                hb = h % 2
                drain("a", 1)

                def q_cons(j, bk, hb=hb):
                    act(qT[:, hb, j, 0:ntok], PS[bk][:, 0:ntok], AF.Copy, scale=1.0 / 16.0, r=[pk(bk)], w=[("qT", hb, j)])
                ws_group(w_in, h * 256, 2, 32, u_rhs, u_keys, ntok, q_cons)

                def k_cons(j, bk, hb=hb):
                    cp(kT[:, hb, j, 0:ntok], PS[bk][:, 0:ntok], r=[pk(bk)], w=[("kT", hb, j)])
                ws_group(w_in, 1024 + h * 256, 2, 32, u_rhs, u_keys, ntok, k_cons)
                for bi, (b0, L) in enumerate(blocks):
                    bk = bank()
                    pb = PS[bk][:].bitcast(BF16)
                    for j in range(2):
                        tr(pb[0:L, j * 128:(j + 1) * 128], kT[:, hb, j, b0:b0 + L], identb, r=[("kT", hb, j), "cb"],
                           w=[pk(bk)], signal=(j == 1))
                    cp(ktok[0:L, hb, bi, :], pb[0:L, 0:256], r=[pk(bk)], w=[("ktok", hb, bi)])
                    free(bk)

                def v_cons(bi, bk, hb=hb):
                    L = blocks[bi][1]
                    cp(vtok[0:L, hb, bi, :], PS[bk][0:L, :], r=[pk(bk)], w=[("vtok", hb, bi)])
                as_group(w_in, 2048 + h * 512, blocks, v_cons)

                def o_cons(bi, bk, hb=hb):
                    L = blocks[bi][1]
                    act(sig[0:L, hb, bi, :], PS[bk][0:L, :], AF.Sigmoid, r=[pk(bk)], w=[("sig", hb, bi)])
                as_group(w_in, 4096 + h * 512, blocks, o_cons)
                lanes["a"].append(chunk_gen(h, hb))

            stage(6)
            def rg_gen(gp, pb_):
                for j in range(2):
                    gi = gp * 2 + j
                    xv = xrh[:, pb_, j, 0:nseg * (Lseg + 3)].rearrange("p (s l) -> p s l", s=nseg)
                    xk = ("xrh", pb_, j); gk_ = ("grb", pb_, j)
                    for s_, slot in enumerate(slots):
                        cp(xv[:, s_, 0:3], cvst[:, slot, gi, :], r=["cvst"], w=[xk])
                    xc3 = xc[:, 0:ntok].rearrange("p (s l) -> p s l", s=nseg)
                    ts(xc3, xv[:, :, 0:Lseg], cw[:, gi * 4:gi * 4 + 1], ALU.mult, convb[:, gi:gi + 1], ALU.add,
                       r=[xk, "vecs"], w=["xc"])
                    for tap in range(1, 4):
                        stt(xc3, xv[:, :, tap:tap + Lseg], cw[:, gi * 4 + tap:gi * 4 + tap + 1], xc3, ALU.mult, ALU.add,
                            r=[xk, "vecs", "xc"], w=["xc"])
                    for s_, slot in enumerate(slots):
                        cp(cvst[:, slot, gi, :], xv[:, s_, Lseg:Lseg + 3], r=[xk], w=["cvst"])
                    act(xcb[:, 0:ntok], xc[:, 0:ntok], AF.Copy, r=["xc"], w=["xcb"])
                    gx = grb[:, pb_, j, 0:ntok]
                    tt(rg_t[:, 0:ntok], gx, gx, ALU.mult, r=[gk_], w=["rg_t"])
                    ts(rg_t[:, 0:ntok], rg_t[:, 0:ntok], 0.044715, ALU.mult, 1.0, ALU.add, r=["rg_t"], w=["rg_t"])
                    tt(rg_t[:, 0:ntok], rg_t[:, 0:ntok], gx, ALU.mult, r=["rg_t", gk_], w=["rg_t"])
                    act(rg_t[:, 0:ntok], rg_t[:, 0:ntok], AF.Sigmoid, scale=1.5957691216057308, r=["rg_t"], w=["rg_t"])
                    tt(rg_t[:, 0:ntok], rg_t[:, 0:ntok], gx, ALU.mult, r=["rg_t", gk_], w=["rg_t"])
                    yield
                    b1 = bank(); b2 = bank()
                    mm(PS[b1][:, 0:ntok], wga[:, gi, :], xcb[:, 0:ntok], True, True, r=["xcb", "wga"], w=[pk(b1)])
                    mm(PS[b2][:, 0:ntok], wgx[:, gi, :], xcb[:, 0:ntok], True, True, r=["xcb", "wgx"], w=[pk(b2)])
                    act(rg_r[:, 0:ntok], PS[b1][:, 0:ntok], AF.Sigmoid, bias=bga[:, gi:gi + 1], r=[pk(b1), "vecs"], w=["rg_r"])
                    act(rg_i[:, 0:ntok], PS[b2][:, 0:ntok], AF.Sigmoid, bias=bgx[:, gi:gi + 1], r=[pk(b2), "vecs"], w=["rg_i"])
                    free(b1, b2)
                    act(rg_a[:, 0:ntok], rg_r[:, 0:ntok], AF.Exp, scale=nsp[:, gi:gi + 1], r=["rg_r", "nsp"], w=["rg_a"])
                    act(rg_m[:, 0:ntok], rg_r[:, 0:ntok], AF.Exp, scale=nsp[:, 16 + gi:17 + gi], r=["rg_r", "nsp"], w=["rg_m"])
                    ts(rg_m[:, 0:ntok], rg_m[:, 0:ntok], -1.0, ALU.mult, 1.0, ALU.add, r=["rg_m"], w=["rg_m"])
                    act(rg_m[:, 0:ntok], rg_m[:, 0:ntok], AF.Sqrt, r=["rg_m"], w=["rg_m"])
                    tt(rg_b[:, 0:ntok], rg_i[:, 0:ntok], xc[:, 0:ntok], ALU.mult, r=["rg_i", "xc"], w=["rg_b"])
                    if first_prompt:
                        cp(sv[:, 20:21], rg_b[:, 0:1], r=["rg_b"], w=["sv20"])
                    tt(rg_b[:, 0:ntok], rg_b[:, 0:ntok], rg_m[:, 0:ntok], ALU.mult, r=["rg_b", "rg_m"], w=["rg_b"])
                    if first_prompt:
                        cp(rg_b[:, 0:1], sv[:, 20:21], r=["sv20"], w=["rg_b"])
                    for s_, slot in enumerate(slots):
                        sl_ = slice(s_ * Lseg, (s_ + 1) * Lseg)
                        c0_ = s_ * Lseg
                        stt(rg_b[:, c0_:c0_ + 1], rg_a[:, c0_:c0_ + 1], hst[:, slot, gi:gi + 1], rg_b[:, c0_:c0_ + 1],
                            ALU.mult, ALU.add, r=["rg_a", "rg_b", "hst"], w=["rg_b"])
                        S.op("dve", lambda e, sl_=sl_: e.tensor_tensor_scan(
                            out=rg_h[:, sl_], data0=rg_a[:, sl_], data1=rg_b[:, sl_],
                            initial=0.0, op0=ALU.mult, op1=ALU.add),
                            ["rg_a", "rg_b"], ["rg_h"])
                        cp(hst[:, slot, gi:gi + 1], rg_h[:, (s_ + 1) * Lseg - 1:(s_ + 1) * Lseg], r=["rg_h"], w=["hst"])
                    tt(Dm[:, 16 + gi, 0:ntok], rg_t[:, 0:ntok], rg_h[:, 0:ntok], ALU.mult, r=["rg_t", "rg_h"], w=[Dk(16 + gi)])
                    yield
                if gp == 7:
                    if kind == "s":
                        dma("sp", oh, hst[:, 1:3, :], r=["hst"])
                        dma("sp", oconv, cvst[:, 1:3], r=["cvst"])
                    elif last_prompt:
                        dma("sp", ph, hst[:, 0, :], r=["hst"])
                        dma("sp", pconv, cvst[:, 0], r=["cvst"])

            def do_pair(gp):
                pb_ = gp % 2
                drain("b", 1)

                def xr_cons(j, bk, pb_=pb_):
                    act(xrh[:, pb_, j, 0:nseg * (Lseg + 3)].rearrange("p (s l) -> p s l", s=nseg)[:, :, 3:3 + Lseg],
                        PS[bk][:, 0:ntok].rearrange("p (s l) -> p s l", s=nseg), AF.Copy, r=[pk(bk)], w=[("xrh", pb_, j)])
                ws_group(w_in, 6152 + gp * 256, 2, 32, u_rhs, u_keys, ntok, xr_cons)

                def gr_cons(j, bk, pb_=pb_):
                    cp(grb[:, pb_, j, 0:ntok], PS[bk][:, 0:ntok], r=[pk(bk)], w=[("grb", pb_, j)])
                ws_group(w_in, 8200 + gp * 256, 2, 32, u_rhs, u_keys, ntok, gr_cons)
                lanes["b"].append(rg_gen(gp, pb_))

            for h in range(4):
                do_head(h)
                do_pair(2 * h)
                do_pair(2 * h + 1)
            drain()

            stage(7)
            d_rhs = lambda k: Dm[:, k, 0:ntok]
            d_keys = lambda k: [Dk(k)]
            for og in range(16):
                def mo_cons(j, bk, og=og):
                    c = og * 2 + j
                    if os.environ.get("MK_VAR") != "1":
                        cp(B[:, c, 0:ntok], PS[bk][:, 0:ntok], r=[pk(bk)], w=[Bk(c)])
                    if os.environ.get("MK_VAR") != "2":
                        accum_sq(c, PS[bk][:, 0:ntok], [pk(bk)], ntok)
                ws_group(w_out, og * 256, 2, 32, d_rhs, d_keys, ntok, mo_cons)
                stage(7.1)
            stage(7.2)
            finish_rstd(ntok, D)
            stage(7.3)
            for c in range(NCH):
                stt(B[:, c, 0:ntok], B[:, c, 0:ntok], gpm[:, c:c + 1], rstd[:, 0:ntok], ALU.mult, ALU.mult,
                    r=[Bk(c), "rstd", "vecs"], w=[Bk(c)])
                tt(A[:, c, 0:ntok], A[:, c, 0:ntok], B[:, c, 0:ntok], ALU.add, r=[Ak(c), Bk(c)], w=[Ak(c)])
            for c in range(NCH):
                accum_sq(c, A[:, c, 0:ntok], [Ak(c)], ntok)
            finish_rstd(ntok, D)
            for c in range(NCH):
                stt(U[:, c, 0:ntok], A[:, c, 0:ntok], gpf[:, c:c + 1], rstd[:, 0:ntok], ALU.mult, ALU.mult,
                    r=[Ak(c), "rstd", "vecs"], w=[Uk(c)])

            stage(8)
            parts = [(0, 15), (15, 14), (29, 14)]
            for pi, (g0, ng) in enumerate(parts):
                for gg in range(ng):
                    col = (g0 + gg) * 256
                    gb = []

                    def g_cons(j, bk):
                        gb.append(bk)
                    ws_group(w_g, col, 2, 32, u_rhs, u_keys, ntok, g_cons, hold=True)

                    def u_cons(j, bk, gg=gg):
                        gk = gb[j]
                        s = sqt[j]
                        act(s[:, 0:ntok], PS[gk][:, 0:ntok], AF.Silu, r=[pk(gk)], w=[("sqt", j)])
                        tt(Dm[:, gg * 2 + j, 0:ntok], s[:, 0:ntok], PS[bk][:, 0:ntok], ALU.mult,
                           r=[("sqt", j), pk(bk)], w=[Dk(gg * 2 + j)])
                        free(gk)
                    ws_group(w_u, col, 2, 32, u_rhs, u_keys, ntok, u_cons)
                K = ng * 2
                for og in range(8):
                    def dn_cons(j, bk, og=og, pi=pi):
                        c = og * 4 + j
                        if pi == 0:
                            cp(B[:, c, 0:ntok], PS[bk][:, 0:ntok], r=[pk(bk)], w=[Bk(c)])
                        else:
                            tt(B[:, c, 0:ntok], B[:, c, 0:ntok], PS[bk][:, 0:ntok], ALU.add, r=[Bk(c), pk(bk)], w=[Bk(c)])
                        if pi == 2:
                            accum_sq(c, B[:, c, 0:ntok], [Bk(c)], ntok)
                    ws_group(w_d[g0 * 256:(g0 + ng) * 256, :], og * 512, 4, K, d_rhs, d_keys, ntok, dn_cons)
            finish_rstd(ntok, D)
            for c in range(NCH):
                stt(B[:, c, 0:ntok], B[:, c, 0:ntok], gpo[:, c:c + 1], rstd[:, 0:ntok], ALU.mult, ALU.mult,
                    r=[Bk(c), "rstd", "vecs"], w=[Bk(c)])
                tt(B[:, c, 0:ntok], B[:, c, 0:ntok], A[:, c, 0:ntok], ALU.add, r=[Ak(c), Bk(c)], w=[Bk(c)])
            stage(9)
            for bi, (b0, L) in enumerate(blocks):
                for pc in range(4):
                    st = stg[pc % 2]; sk = ("stg", pc % 2)
                    for half in range(2):
                        bk = bank()
                        for j in range(4):
                            c = pc * 8 + half * 4 + j
                            tr(PS[bk][0:L, j * 128:(j + 1) * 128], B[:, c, b0:b0 + L], ident, r=[Bk(c), "cf"],
                               w=[pk(bk)], signal=(j == 3))
                        evac_copy(st[0:L, half * 512:(half + 1) * 512], PS[bk][0:L, :], r=[pk(bk)], w=[sk])
                        free(bk)
                    dma("sp", yd[row0 + b0:row0 + b0 + L, pc * 1024:(pc + 1) * 1024], st[0:L, :], r=[sk])

        try:
            do_tile(n_ptiles, "s")
            for ti in range(n_ptiles):
                do_tile(ti, "p")
        except StopBuild:
            pass
        S.finish()

        sems = {sk: nc.alloc_semaphore(name=f"s_{sk[0]}_{sk[1]}") for sk in sorted(S.semkeys, key=str)}
        engs = {"pe": "tensor", "act": "scalar", "dve": "vector", "pool": "gpsimd", "sp": "sync"}

        def replay(e, name):
            for waits, fn, sig_ in S.ops[name]:
                for sk, v in waits:
                    e.wait_ge(sems[sk], v)
                if fn is None:
                    continue
                ins = fn(e)
                if sig_ is not None:
                    ins.then_inc(sems[sig_[0]], sig_[1])

        with nc.Block() as block:
            @block.tensor
            def _(e):
                replay(e, "pe")

            @block.scalar
            def _(e):
                replay(e, "act")

            @block.vector
            def _(e):
                replay(e, "dve")

            @block.gpsimd
            def _(e):
                replay(e, "pool")

            @block.sync
            def _(e):
                replay(e, "sp")
    return nc


def _consts():
    idx = np.arange(128)
    ident = np.eye(128, dtype=np.float32)
    tri = (idx[:, None] <= idx[None, :]).astype(np.float32)
    maskA = np.where(idx[None, :] <= idx[:, None], 0.0, -BIG).astype(np.float32)
    maskB = np.where(idx[:, None] <= idx[None, :], 0.0, BIG).astype(np.float32)
    ones = np.ones((128, 128), np.float32)
    cst = np.zeros((128, 8), np.float32)
    cst[:, 0] = EPS
    cst[:, 1] = 1.0
    cf = np.concatenate([ident, tri, maskA, maskB, ones, cst], axis=1)
    cb = np.concatenate([ident, ones], axis=1).astype(ml_dtypes.bfloat16)
    return np.ascontiguousarray(cf), np.ascontiguousarray(cb)


def _fm(v, nchunks):
    return np.ascontiguousarray(np.asarray(v, np.float32).reshape(nchunks, 128).T)


_NC_CACHE = {}


def _run(inputs, n_cores, n_ptiles):
    f = lambda k: np.asarray(inputs[k], np.float32)
    xp_all = f("x_prompt"); xs_all = f("x_sample")
    SEQ = n_ptiles * NT
    assert xp_all.shape[1] == SEQ
    if n_ptiles not in _NC_CACHE:
        _NC_CACHE[n_ptiles] = build_nc(n_ptiles)
    nc = _NC_CACHE[n_ptiles]
    cf, cb = _consts()
    vecs = np.zeros((128, 352), np.float32)
    vecs[:, 0:32] = _fm(f("g_pre_mix")[0], 32); vecs[:, 32:64] = _fm(f("g_post_mix")[0], 32)
    vecs[:, 64:96] = _fm(f("g_pre_ffn")[0], 32); vecs[:, 96:128] = _fm(f("g_post_ffn")[0], 32)
    vecs[:, 128:144] = _fm(f("g_mlstm_head")[0], 16)
    cwv = f("conv_w")[0]
    vecs[:, 144:208] = cwv.reshape(4, 16, 128).transpose(2, 1, 0).reshape(128, 64)
    vecs[:, 208:224] = _fm(f("conv_b")[0], 16); vecs[:, 224:240] = _fm(f("b_rg_a")[0], 16)
    vecs[:, 240:256] = _fm(f("b_rg_x")[0], 16); vecs[:, 256:272] = _fm(f("rg_lambda")[0], 16)
    bif = np.ascontiguousarray(np.broadcast_to(
        np.concatenate([f("b_igate")[0], f("b_fgate")[0]])[None, :], (128, 8)))
    shared = {
        "w_in": f("w_in")[0], "w_out": f("w_out")[0], "w_g": f("w_ffn_gate")[0], "w_u": f("w_ffn_up")[0],
        "w_d": f("w_ffn_down")[0], "w_rga": f("w_rg_a")[0], "w_rgx": f("w_rg_x")[0],
        "vecs": vecs, "bif": bif, "cf": cf, "cb": cb,
    }
    sC = f("state_mlstm_C")[0]; sn = f("state_mlstm_n")[0]; sm = f("state_mlstm_m")[0]
    sh = f("state_rglru_h")[0]; scv = f("state_rglru_conv")[0]
    in_maps = []
    for c in range(n_cores):
        sl = slice(2 * c, 2 * c + 2)
        m = dict(shared)
        m["xp"] = np.ascontiguousarray(xp_all[c])
        m["xs"] = np.ascontiguousarray(xs_all[sl].reshape(64, D))
        m["sC"] = np.ascontiguousarray(sC[sl])
        m["sn"] = np.ascontiguousarray(sn[sl].reshape(2, 4, 2, 128).transpose(3, 0, 1, 2))
        m["sm"] = np.ascontiguousarray(np.broadcast_to(sm[sl][None], (128, 2, 4)))
        m["sh"] = np.ascontiguousarray(sh[sl].reshape(2, 16, 128).transpose(2, 0, 1))
        m["sconv"] = np.ascontiguousarray(scv[sl].reshape(2, 3, 16, 128).transpose(3, 0, 2, 1))
        in_maps.append(m)
    res = run_bass_kernel_spmd(nc, in_maps, core_ids=list(range(n_cores)))
    R = res.results
    B_, DB = n_cores, 2 * n_cores
    y_p = np.stack([R[c]["yp"] for c in range(B_)])
    y_s = np.concatenate([R[c]["ys"].reshape(2, 32, D) for c in range(B_)])
    p_C = np.stack([R[c]["pC"] for c in range(B_)])[None]
    p_n = np.stack([R[c]["pn"].transpose(1, 2, 0).reshape(4, 256) for c in range(B_)])[None]
    p_m = np.stack([R[c]["pm"][0] for c in range(B_)])[None]
    p_h = np.stack([R[c]["ph"].T.reshape(2048) for c in range(B_)])[None]
    p_conv = np.stack([R[c]["pconv"].transpose(2, 1, 0).reshape(3, 2048) for c in range(B_)])[None]
    s_C = np.concatenate([R[c]["oC"] for c in range(B_)])[None]
    s_n = np.concatenate([R[c]["on"].transpose(1, 2, 3, 0).reshape(2, 4, 256) for c in range(B_)])[None]
    s_m = np.concatenate([R[c]["om"][0] for c in range(B_)])[None]
    s_h = np.concatenate([R[c]["oh"].transpose(1, 2, 0).reshape(2, 2048) for c in range(B_)])[None]
    s_conv = np.concatenate([R[c]["oconv"].transpose(1, 3, 2, 0).reshape(2, 3, 2048) for c in range(B_)])[None]
    outs = (y_p, y_s, p_C, p_n, p_m, p_h, p_conv, s_C, s_n, s_m, s_h, s_conv)
    return tuple(np.ascontiguousarray(o, dtype=np.float32) for o in outs)


def kernel(**inputs):
    return _run(inputs, 8, 8)
```

```python
import contextlib
import os
import numpy as np
import ml_dtypes
import concourse.bass as bass
import concourse.mybir as mybir
from concourse.bass_utils import run_bass_kernel_spmd

F32 = mybir.dt.float32
BF16 = mybir.dt.bfloat16
AF = mybir.ActivationFunctionType
ALU = mybir.AluOpType
AX = mybir.AxisListType

D = 4096
NCH = 32
DFF = 11008
DIN = 10248
NT = 256
BIG = 30000.0
EPS = 1e-6
NWBUF = 3
COMPUTE = ("pe", "act", "dve", "pool")
MK_STAGE = float(os.environ.get("MK_STAGE", "99"))


class StopBuild(Exception):
    pass


def stage(n):
    if MK_STAGE < n:
        raise StopBuild()


class Sched:
    def __init__(self):
        self.ops = {e: [] for e in ("pe", "act", "dve", "pool", "sp")}
        self.cnt = {}
        self.epoch = 0
        self.waited = {e: {} for e in self.ops}
        self.lastw = {}
        self.readers = {}
        self.dma_pool = {"sp": [("dsp", i) for i in range(8)], "pool": [("dpl", i) for i in range(6)]}
        self.dma_rr = {"sp": 0, "pool": 0}
        self.dma_last = {}
        self.semkeys = set()

    def cur(self, eng):
        return (eng, self.epoch)

    def _deps(self, reads, writes):
        deps = []
        for k in reads:
            t = self.lastw.get(k)
            if t is not None:
                deps.append(t)
        for k in writes:
            t = self.lastw.get(k)
            if t is not None:
                deps.append(t)
            r = self.readers.get(k)
            if r:
                deps.extend(r.items())
        return deps

    def _filter(self, eng, deps):
        best = {}
        w = self.waited[eng]
        for sk, v in deps:
            if eng == "pe" and sk[0] == "pe":
                continue
            if w.get(sk, 0) >= v:
                continue
            if best.get(sk, 0) < v:
                best[sk] = v
        for sk, v in best.items():
            w[sk] = v
        return list(best.items())

    def _register(self, tok, reads, writes):
        sk, v = tok
        for k in reads:
            r = self.readers.setdefault(k, {})
            if r.get(sk, 0) < v:
                r[sk] = v
        for k in writes:
            self.lastw[k] = tok
            self.readers[k] = {}

    def op(self, eng, fn, reads=(), writes=(), signal=True):
        assert signal or eng == "pe"
        psr = [k for k in reads if isinstance(k, tuple) and k[0] == "ps"]
        if psr:
            reads = [k for k in reads if k not in psr]
            writes = list(writes) + psr
        waits = self._filter(eng, self._deps(reads, writes))
        sk = self.cur(eng)
        self.semkeys.add(sk)
        val = self.cnt.get(sk, 0) + 1
        if signal:
            self.cnt[sk] = val
        self._register((sk, val), reads, writes)
        self.ops[eng].append((waits, fn, (sk, 1) if signal else None))

    def dma(self, q, fn, reads=(), writes=()):
        deps = self._deps(reads, writes)
        pool = self.dma_pool[q]
        sk = pool[self.dma_rr[q] % len(pool)]
        self.dma_rr[q] += 1
        self.semkeys.add(sk)
        prev = self.dma_last.get(sk)
        if prev:
            deps.append((sk, prev))
        waits = self._filter(q, deps)
        val = self.cnt.get(sk, 0) + 16
        self.cnt[sk] = val
        self.dma_last[sk] = val
        self._register((sk, val), reads, writes)
        self.ops[q].append((waits, fn, (sk, 16)))

    def finish(self):
        waits = self._filter("sp", [(sk, v) for sk, v in self.dma_last.items()])
        self.ops["sp"].append((waits, None, None))


def build_nc(n_ptiles):
    SEQ = n_ptiles * NT
    nc = bass.Bass("TRN2", target_bir_lowering=False)
    S = Sched()

    def din(name, shape, dt=F32):
        return nc.dram_tensor(name, list(shape), dt, kind="ExternalInput").ap()

    def dout(name, shape, dt=F32):
        return nc.dram_tensor(name, list(shape), dt, kind="ExternalOutput").ap()

    xp = din("xp", [SEQ, D]); xs = din("xs", [64, D])
    sC_in = din("sC", [2, 4, 256, 512]); sn_in = din("sn", [128, 2, 4, 2]); sm_in = din("sm", [128, 2, 4])
    sh_in = din("sh", [128, 2, 16]); sconv_in = din("sconv", [128, 2, 16, 3])
    w_in = din("w_in", [D, DIN]); w_out = din("w_out", [D, D])
    w_g = din("w_g", [D, DFF]); w_u = din("w_u", [D, DFF]); w_d = din("w_d", [DFF, D])
    w_rga = din("w_rga", [16, 128, 128]); w_rgx = din("w_rgx", [16, 128, 128])
    vecs_d = din("vecs", [128, 352])
    bif_d = din("bif", [128, 8])
    cf_d = din("cf", [128, 5 * 128 + 8])
    cb_d = din("cb", [128, 256], BF16)

    yp = dout("yp", [SEQ, D]); ys = dout("ys", [64, D])
    pC = dout("pC", [4, 256, 512]); pn = dout("pn", [128, 4, 2]); pm = dout("pm", [128, 4])
    ph = dout("ph", [128, 16]); pconv = dout("pconv", [128, 16, 3])
    oC = dout("oC", [2, 4, 256, 512]); on = dout("on", [128, 2, 4, 2]); om = dout("om", [128, 2, 4])
    oh = dout("oh", [128, 2, 16]); oconv = dout("oconv", [128, 2, 16, 3])

    SCR_PER = 64
    scr = [nc.dram_tensor(f"wscr{i}", [SCR_PER, 128, 4096], BF16, kind="Internal").ap() for i in range(6)]

    es = contextlib.ExitStack()

    def sb(name, shape, dt=F32):
        return es.enter_context(nc.sbuf_tensor(name, list(shape), dt))

    with es:
        A = sb("A", [128, NCH, NT]); B = sb("B", [128, NCH, NT])
        U = sb("U", [128, NCH, NT], BF16); Dm = sb("Dm", [128, NCH, NT], BF16)
        W = [sb(f"W{i}", [128, 4096], BF16) for i in range(NWBUF)]
        Cst = sb("Cst", [128, 4, 2, 512]); nst = sb("nst", [128, 4, 2]); mst = sb("mst", [128, 4])
        hst = sb("hst", [128, 3, 16]); cvst = sb("cvst", [128, 3, 16, 3])
        wif = sb("wif", [128, 32, 8], BF16)
        wga = sb("wga", [128, 16, 128], BF16); wgx = sb("wgx", [128, 16, 128], BF16)
        vecs = sb("vecs_s", [128, 352]); bif = sb("bif_s", [128, 8])
        cf = sb("cf_s", [128, 5 * 128 + 8]); cb = sb("cb_s", [128, 256], BF16)
        nsp = sb("nsp", [128, 32])
        stg = [sb(f"stg{i}", [128, 1024]) for i in range(2)]
        acc = sb("acc", [128, NT]); sqt = [sb(f"sqt{i}", [128, NT]) for i in range(2)]
        rstd = sb("rstd", [128, NT]); lnv = sb("lnv", [128, NT])
        qT = sb("qT", [128, 2, 2, NT], BF16); kT = sb("kT", [128, 2, 2, NT], BF16)
        ktok = sb("ktok", [128, 2, 2, 256], BF16); vtok = sb("vtok", [128, 2, 2, 512], BF16)
        sig = sb("sig", [128, 2, 2, 512], BF16)
        igf = sb("igf", [128, 2, 8]); sp8 = sb("sp8", [128, 2, 8]); cvec = sb("cvec", [128, 2, 4])
        nb = sb("nb", [128, 2, 4]); nbl = sb("nbl", [128, 2, 4])
        dg = sb("dg", [128, 128]); dg2 = sb("dg2", [128, 128]); tll = sb("tll", [128, 128])
        dmT = sb("dmT", [128, 128]); PT = sb("PT", [128, 128], BF16)
        t1 = sb("t1", [128, 512]); t2 = sb("t2", [128, 512]); t4 = sb("t4", [128, 512], BF16)
        kw = sb("kw", [128, 256], BF16); Cbf = sb("Cbf", [128, 2, 512], BF16); nbf = sb("nbf", [128, 2], BF16)
        sv = sb("sv", [128, 24])
        xrh = sb("xrh", [128, 2, 2, NT + 8]); grb = sb("grb", [128, 2, 2, NT])
        xc = sb("xc", [128, NT]); xcb = sb("xcb", [128, NT], BF16)
        rg_r = sb("rg_r", [128, NT]); rg_i = sb("rg_i", [128, NT]); rg_a = sb("rg_a", [128, NT])
        rg_m = sb("rg_m", [128, NT]); rg_b = sb("rg_b", [128, NT]); rg_h = sb("rg_h", [128, NT])
        rg_t = sb("rg_t", [128, NT])
        PS = [es.enter_context(nc.psum_tensor(f"ps{i}", [128, 512], F32)) for i in range(8)]

        ident = cf[:, 0:128]; tri = cf[:, 128:256]; maskA = cf[:, 256:384]; maskB = cf[:, 384:512]
        ones = cf[:, 512:640]; cst = cf[:, 640:648]
        eps_ap = cst[:, 0:1]; one_ap = cst[:, 1:2]
        identb = cb[:, 0:128]; onesb = cb[:, 128:256]
        gpre = vecs[:, 0:32]; gpm = vecs[:, 32:64]; gpf = vecs[:, 64:96]; gpo = vecs[:, 96:128]
        ghead = vecs[:, 128:144]; cw = vecs[:, 144:208]; convb = vecs[:, 208:224]
        bga = vecs[:, 224:240]; bgx = vecs[:, 240:256]; lam = vecs[:, 256:272]

        free_banks = list(range(8))

        def bank():
            assert free_banks, "PSUM banks exhausted"
            return free_banks.pop(0)

        def free(*bs):
            for b in bs:
                assert b not in free_banks
                free_banks.append(b)

        def pk(i):
            return ("ps", i)

        def act(out, in_, func, bias=None, scale=None, accum=None, r=(), w=()):
            def fn(e):
                kw_ = {}
                if bias is not None:
                    kw_["bias"] = bias
                if scale is not None:
                    kw_["scale"] = scale
                if accum is not None:
                    kw_["accum_out"] = accum
                return e.activation(out=out, in_=in_, func=func, **kw_)
            S.op("act", fn, r, w)

        def tt(out, in0, in1, op, r=(), w=(), eng="dve"):
            S.op(eng, lambda e: e.tensor_tensor(out=out, in0=in0, in1=in1, op=op), r, w)

        def ts(out, in0, s1, op0, s2=None, op1=None, r=(), w=(), eng="dve"):
            def fn(e):
                if op1 is None:
                    return e.tensor_scalar(out=out, in0=in0, scalar1=s1, scalar2=None, op0=op0)
                return e.tensor_scalar(out=out, in0=in0, scalar1=s1, scalar2=s2, op0=op0, op1=op1)
            S.op(eng, fn, r, w)

        def stt(out, in0, sc, in1, op0, op1, r=(), w=()):
            S.op("dve", lambda e: e.scalar_tensor_tensor(out=out, in0=in0, scalar=sc, in1=in1, op0=op0, op1=op1), r, w)

        def cp(out, in_, r=(), w=(), eng="dve"):
            S.op(eng, lambda e: e.tensor_copy(out=out, in_=in_), r, w)

        def mm(out, lhsT, rhs, start, stop, r=(), w=(), signal=None):
            S.op("pe", lambda e: e.matmul(out, lhsT=lhsT, rhs=rhs, start=start, stop=stop), r, w,
                 signal=stop if signal is None else signal)

        def tr(out, in_, idn, r=(), w=(), signal=True):
            S.op("pe", lambda e: e.transpose(out=out, in_=in_, identity=idn), r, w, signal=signal)

        def dma(q, out, in_, r=(), w=(), nonc=False):
            def fn(e):
                with nc.allow_non_contiguous_dma(reason="small"):
                    return e.dma_start(out=out, in_=in_)
            S.dma(q, fn, r, w)

        evrr = [0]

        def evac_copy(out, in_, r, w):
            evrr[0] += 1
            if evrr[0] % 2:
                cp(out, in_, r, w)
            else:
                act(out, in_, AF.Copy, r=r, w=w)

        lanes = {"a": [], "b": []}

        def tick1(q):
            while q:
                try:
                    next(q[0])
                    return
                except StopIteration:
                    q.pop(0)

        tick_n = [0]

        def tick():
            tick_n[0] += 1
            if tick_n[0] % 2:
                return
            tick1(lanes["a"])
            tick1(lanes["b"])

        def drain(lane=None, maxlen=0):
            for nm in ([lane] if lane else ["a", "b"]):
                q = lanes[nm]
                while len(q) > maxlen:
                    tick1(q)
                    if lane is None or True:
                        other = lanes["b" if nm == "a" else "a"]
                        tick1(other)

        wrr = [0]

        slab_idx = [0]
        slab_mode = ["cast"]

        def slab(wd, r0, nk, c0, ncol):
            i = wrr[0] % NWBUF
            wrr[0] += 1
            si = slab_idx[0]
            slab_idx[0] += 1
            flat = W[i][:, 0:nk * ncol]
            view = flat.rearrange("p (k c) -> p k c", k=nk)
            sdst = scr[si // SCR_PER][si % SCR_PER][:, 0:nk * ncol]
            if slab_mode[0] == "cast":
                src = wd[r0:r0 + nk * 128, c0:c0 + ncol].rearrange("(k p) c -> p k c", p=128)
                dma("pool", view, src, w=[("W", i)])
                dma("sp", sdst, flat, r=[("W", i)], w=[("scr", si)])
            else:
                dma("pool", flat, sdst, r=[("scr", si)], w=[("W", i)])
            return view, ("W", i)

        def ws_group(wd, c0, nch, K, rhs_of, rkeys_of, ncols, consume, hold=False):
            ncol = nch * 128
            nk_max = 4096 // ncol
            banks = [bank() for _ in range(nch)]
            k0 = 0
            while k0 < K:
                nk = min(nk_max, K - k0)
                view, wkey = slab(wd, k0 * 128, nk, c0, ncol)
                for j in range(nch):
                    for kk in range(nk):
                        k = k0 + kk
                        last_in_slab = (j == nch - 1 and kk == nk - 1)
                        mm(PS[banks[j]][:, 0:ncols], view[:, kk, j * 128:(j + 1) * 128], rhs_of(k),
                           start=(k == 0), stop=(k == K - 1), r=[wkey] + rkeys_of(k), w=[pk(banks[j])],
                           signal=(k == K - 1) or last_in_slab)
                k0 += nk
                tick()
            for j in range(nch):
                consume(j, banks[j])
                if not hold:
                    free(banks[j])

        def as_group(wd, c0, blocks, consume):
            banks = [bank() for _ in blocks]
            for k0 in range(0, 32, 8):
                view, wkey = slab(wd, k0 * 128, 8, c0, 512)
                for bi, (b0, L) in enumerate(blocks):
                    for kk in range(8):
                        k = k0 + kk
                        last_in_slab = (bi == len(blocks) - 1 and kk == 7)
                        mm(PS[banks[bi]][0:L, :], U[:, k, b0:b0 + L], view[:, kk, :],
                           start=(k == 0), stop=(k == 31), r=[wkey, ("U", k)], w=[pk(banks[bi])],
                           signal=(k == 31) or last_in_slab)
                tick()
            for bi in range(len(blocks)):
                consume(bi, banks[bi])
                free(banks[bi])

        def finish_rstd(ntok, dim):
            bi = bank()
            mm(PS[bi][:, 0:ntok], ones, acc[:, 0:ntok], True, True, r=["acc"], w=[pk(bi)])
            act(lnv[:, 0:ntok], PS[bi][:, 0:ntok], AF.Ln, bias=eps_ap, scale=1.0 / dim, r=[pk(bi)], w=["lnv"])
            act(rstd[:, 0:ntok], lnv[:, 0:ntok], AF.Exp, scale=-0.5, r=["lnv"], w=["rstd"])
            free(bi)

        def accum_sq(c, src, srckeys, ntok):
            if c == 0:
                act(acc[:, 0:ntok], src, AF.Square, r=srckeys, w=["acc"])
            else:
                s = sqt[c % 2]
                act(s[:, 0:ntok], src, AF.Square, r=srckeys, w=[("sqt", c % 2)])
                tt(acc[:, 0:ntok], acc[:, 0:ntok], s[:, 0:ntok], ALU.add, r=[("sqt", c % 2), "acc"], w=["acc"])

        dma("sp", cf[:], cf_d, w=["cf"]); dma("sp", cb[:], cb_d, w=["cb"])
        dma("sp", vecs[:], vecs_d, w=["vecs"]); dma("sp", bif[:], bif_d, w=["bif"])
        dma("pool", wif[:], w_in[:, 6144:6152].rearrange("(k p) c -> p k c", p=128), w=["wif"], nonc=True)
        dma("pool", wga[:], w_rga.rearrange("g i j -> i g j"), w=["wga"])
        dma("pool", wgx[:], w_rgx.rearrange("g i j -> i g j"), w=["wgx"])
        act(nsp[:, 0:16], lam, AF.Exp, scale=-1.0, r=["vecs"], w=["nsp"])
        act(nsp[:, 16:32], nsp[:, 0:16], AF.Ln, bias=one_ap, r=["nsp", "cf"], w=["nsp"])
        ts(nsp[:, 0:16], nsp[:, 16:32], -8.0, ALU.mult, r=["nsp"], w=["nsp"])
        ts(nsp[:, 16:32], nsp[:, 16:32], -16.0, ALU.mult, r=["nsp"], w=["nsp"])
        S.op("dve", lambda e: e.memset(hst[:, 0, :], 0.0), (), ["hst"])
        S.op("dve", lambda e: e.memset(cvst[:, 0].rearrange("p a b -> p (a b)"), 0.0), (), ["cvst"])
        dma("sp", hst[:, 1:3, :], sh_in, w=["hst"])
        dma("sp", cvst[:, 1:3], sconv_in, w=["cvst"])
        const_keys = ["cf", "cb", "vecs", "bif", "wif", "wga", "wgx", "nsp"]

        def do_tile(ti, kind):
            S.epoch = ti + 1 if kind == "p" else 0
            slab_idx[0] = 0
            slab_mode[0] = "cast" if kind == "s" else "scr"
            if kind == "p" and ti == 0:
                S.op("dve", lambda e: e.memset(Cst[:].rearrange("p a b c -> p (a b c)"), 0.0), (), [("Cst", h) for h in range(4)])
                S.op("dve", lambda e: e.memset(nst[:].rearrange("p a b -> p (a b)"), 0.0), (), ["nst"])
                S.op("dve", lambda e: e.memset(mst[:], 0.0), (), ["mst"])
            if kind == "p":
                xd, yd, row0 = xp, yp, ti * NT
                blocks = [(0, 128), (128, 128)]
                nseg, Lseg, ntok = 1, NT, NT
                slots = [0]
            else:
                xd, yd, row0 = xs, ys, 0
                blocks = [(0, 32), (32, 32)]
                nseg, Lseg, ntok = 2, 32, 64
                slots = [1, 2]
            first_prompt = (kind == "p" and ti == 0)
            last_prompt = (kind == "p" and ti == n_ptiles - 1)
            Ak = lambda c: ("A", c)
            Bk = lambda c: ("B", c)
            Uk = lambda c: ("U", c)
            Dk = lambda c: ("D", c)

            for bi, (b0, L) in enumerate(blocks):
                for pc in range(4):
                    st = stg[pc % 2]; sk = ("stg", pc % 2)
                    dma("sp", st[0:L, :], xd[row0 + b0:row0 + b0 + L, pc * 1024:(pc + 1) * 1024], w=[sk])
                    for half in range(2):
                        bk = bank()
                        for j in range(4):
                            tr(PS[bk][:, j * L:(j + 1) * L], st[0:L, (half * 4 + j) * 128:(half * 4 + j + 1) * 128],
                               ident[0:L, 0:L], r=[sk, "cf"], w=[pk(bk)], signal=(j == 3))
                        c0 = pc * 8 + half * 4
                        evac_copy(A[:, c0:c0 + 4, b0:b0 + L], PS[bk][:, 0:4 * L].rearrange("p (j l) -> p j l", j=4),
                                  r=[pk(bk)], w=[Ak(c0 + j) for j in range(4)])
                        free(bk)
            stage(1)
            for c in range(NCH):
                accum_sq(c, A[:, c, 0:ntok], [Ak(c)], ntok)
            finish_rstd(ntok, D)
            for c in range(NCH):
                stt(U[:, c, 0:ntok], A[:, c, 0:ntok], gpre[:, c:c + 1], rstd[:, 0:ntok], ALU.mult, ALU.mult,
                    r=[Ak(c), "rstd", "vecs"], w=[Uk(c)])

            u_rhs = lambda k: U[:, k, 0:ntok]
            u_keys = lambda k: [Uk(k)]

            stage(2)
            for bi, (b0, L) in enumerate(blocks):
                bk = bank()
                for k in range(32):
                    mm(PS[bk][0:L, 0:8], U[:, k, b0:b0 + L], wif[:, k, :], k == 0, k == 31,
                       r=[Uk(k), "wif"], w=[pk(bk)])
                tt(igf[0:L, bi, :], PS[bk][0:L, 0:8], bif[0:L, :], ALU.add, r=[pk(bk), "bif"], w=[("igf", bi)])
                free(bk)
                act(sp8[0:L, bi, 0:4], igf[0:L, bi, 4:8], AF.Exp, scale=-1.0, r=[("igf", bi)], w=[("sp8", bi)])
                act(sp8[0:L, bi, 4:8], sp8[0:L, bi, 0:4], AF.Ln, bias=one_ap[0:L], r=[("sp8", bi), "cf"], w=[("sp8", bi)])
                bk2 = bank()
                mm(PS[bk2][0:L, 0:4], tri[0:L, 0:L], sp8[0:L, bi, 4:8], True, True, r=[("sp8", bi), "cf"], w=[pk(bk2)])
                mm(PS[bk2][:, 4:8], ones[0:L, :], sp8[0:L, bi, 4:8], True, True, r=[("sp8", bi), "cf"], w=[pk(bk2)])
                cp(nb[0:L, bi, :], PS[bk2][0:L, 0:4], r=[pk(bk2)], w=[("nb", bi)])
                cp(nbl[:, bi, :], PS[bk2][:, 4:8], r=[pk(bk2)], w=[("nbl", bi)])
                free(bk2)
                tt(cvec[0:L, bi, :], igf[0:L, bi, 0:4], nb[0:L, bi, :], ALU.add, r=[("igf", bi), ("nb", bi)], w=[("cvec", bi)])

            stage(3)
            def chunk_gen(h, hb):
                for bi, (b0, L) in enumerate(blocks):
                    if kind == "p":
                        slot = h
                    else:
                        slot = (2 * h + bi) % 4
                        dma("sp", Cst[:, slot], sC_in[bi, h].rearrange("(j p) e -> p j e", p=128), w=[("Cst", slot)])
                        dma("sp", nst[:, slot, :], sn_in[:, bi, h, :], w=["nst"])
                        dma("sp", mst[:, slot:slot + 1], sm_in[:, bi, h:h + 1], w=["mst"])
                    Ck = ("Cst", slot)
                    qk = lambda j: ("qT", hb, j)
                    kk_ = lambda j: ("kT", hb, j)
                    vk = ("vtok", hb, bi); sgk = ("sig", hb, bi); ktk = ("ktok", hb, bi)
                    c_h = cvec[0:L, bi, h:h + 1]
                    m_b = mst[:, slot:slot + 1]
                    cm = sv[0:L, 0:1]; g = sv[0:L, 1:2]; glb = sv[:, 2:3]; nglb = sv[:, 3:4]
                    winter = sv[0:L, 4:5]; r2 = sv[0:L, 5:7]; den = sv[0:L, 7:8]; mj = sv[0:L, 8:9]
                    emj = sv[0:L, 9:10]; rden = sv[0:L, 10:11]; ss = sv[0:L, 11:12]; vv = sv[0:L, 12:13]
                    sc = sv[0:L, 13:14]; wsv = sv[0:L, 14:15]; wC = sv[:, 15:16]
                    bs = bank()
                    for j in range(2):
                        mm(PS[bs][0:L, 0:L], kT[:, hb, j, b0:b0 + L], qT[:, hb, j, b0:b0 + L], j == 0, j == 1,
                           r=[kk_(j), qk(j)], w=[pk(bs)])
                    cp(Cbf[:].rearrange("p a b -> p (a b)"), Cst[:, slot].rearrange("p a b -> p (a b)"), r=[Ck], w=["Cbf"])
                    cp(nbf[:], nst[:, slot, :], r=["nst"], w=["nbf"])
                    ts(dg[0:L, 0:L], ident[0:L, 0:L], c_h, ALU.mult, r=[("cvec", bi), "cf"], w=["dg"])
                    yield
                    bx = bank()
                    mm(PS[bx][0:L, 0:L], ones[0:L, 0:L], dg[0:L, 0:L], True, True, r=["dg", "cf"], w=[pk(bx)])
                    tt(tll[0:L, 0:L], PS[bx][0:L, 0:L], maskA[0:L, 0:L], ALU.add, r=[pk(bx), "cf"], w=["tll"])
                    free(bx)
                    S.op("dve", lambda e, cm=cm, L=L: e.reduce_max(out=cm, in_=tll[0:L, 0:L], axis=AX.X), ["tll"], ["sv"])
                    tt(g, cm, m_b[0:L], ALU.max, r=["sv", "mst"], w=["sv"])
                    ts(dg2[0:L, 0:L], ident[0:L, 0:L], g, ALU.mult, r=["sv", "cf"], w=["dg2"])
                    yield
                    by = bank()
                    mm(PS[by][:, 0:L], ones[0:L, :], dg2[0:L, 0:L], True, True, r=["dg2", "cf"], w=[pk(by)])
                    tt(tll[0:L, 0:L], PS[by][0:L, 0:L], maskB[0:L, 0:L], ALU.add, r=[pk(by), "cf"], w=["tll"])
                    cp(glb, PS[by][:, L - 1:L], r=[pk(by)], w=["sv"])
                    free(by)
                    act(dmT[0:L, 0:L], tll[0:L, 0:L], AF.Exp, bias=c_h, scale=-1.0, r=["tll", ("cvec", bi)], w=["dmT"])
                    ts(nglb, glb, -1.0, ALU.mult, r=["sv"], w=["sv"])
                    tt(PT[0:L, 0:L], PS[bs][0:L, 0:L], dmT[0:L, 0:L], ALU.mult, r=[pk(bs), "dmT"], w=["PT"])
                    free(bs)
                    act(winter, g, AF.Exp, bias=m_b[0:L], scale=-1.0, r=["sv", "mst"], w=["sv"])
                    tt(mj, g, nb[0:L, bi, h:h + 1], ALU.subtract, r=["sv", ("nb", bi)], w=["sv"])
                    act(emj, mj, AF.Exp, scale=-1.0, r=["sv"], w=["sv"])
                    act(wsv, c_h, AF.Exp, bias=nglb[0:L], r=[("cvec", bi), "sv"], w=["sv"])
                    act(wC, m_b, AF.Exp, bias=nglb, r=["mst", "sv"], w=["sv"])
                    ts(kw[0:L, :], ktok[0:L, hb, bi, :], wsv, ALU.mult, r=[ktk, "sv"], w=["kw"])
                    yield
                    bn = bank()
                    mm(PS[bn][0:L, :], PT[0:L, 0:L], vtok[0:L, hb, bi, :], True, True, r=["PT", vk], w=[pk(bn)])
                    bi_ = bank()
                    for j in range(2):
                        mm(PS[bi_][0:L, :], qT[:, hb, j, b0:b0 + L], Cbf[:, j, :], j == 0, j == 1,
                           r=[qk(j), "Cbf"], w=[pk(bi_)])
                    br = bank()
                    mm(PS[br][0:L, 0:1], PT[0:L, 0:L], onesb[0:L, 0:1], True, True, r=["PT", "cb"], w=[pk(br)])
                    for j in range(2):
                        mm(PS[br][0:L, 1:2], qT[:, hb, j, b0:b0 + L], nbf[:, j:j + 1], j == 0, j == 1,
                           r=[qk(j), "nbf"], w=[pk(br)])
                    bc = [bank(), bank()]
                    for j in range(2):
                        mm(PS[bc[j]][:, :], kw[0:L, j * 128:(j + 1) * 128], vtok[0:L, hb, bi, :], True, True,
                           r=["kw", vk], w=[pk(bc[j])])
                    bd = br
                    for j in range(2):
                        mm(PS[bd][:, 2 + j:3 + j], kw[0:L, j * 128:(j + 1) * 128], onesb[0:L, 0:1], True, True,
                           r=["kw", "cb"], w=[pk(bd)])
                    cp(r2, PS[br][0:L, 0:2], r=[pk(br)], w=["sv"])
                    stt(den, r2[:, 1:2], winter, r2[:, 0:1], ALU.mult, ALU.add, r=["sv"], w=["sv"])
                    stt(den, den, -1.0, den, ALU.mult, ALU.max, r=["sv"], w=["sv"])
                    tt(den, den, emj, ALU.max, r=["sv"], w=["sv"])
                    S.op("dve", lambda e, rden=rden, den=den: e.reciprocal(out=rden, in_=den), ["sv"], ["sv"])
                    act(t1[0:L, :], PS[bi_][0:L, :], AF.Copy, scale=winter, r=[pk(bi_), "sv"], w=["t1"])
                    tt(t2[0:L, :], PS[bn][0:L, :], t1[0:L, :], ALU.add, r=[pk(bn), "t1"], w=["t2"])
                    for j in range(2):
                        stt(Cst[:, slot, j, :], Cst[:, slot, j, :], wC, PS[bc[j]][:, :], ALU.mult, ALU.add,
                            r=[Ck, "sv", pk(bc[j])], w=[Ck])
                    stt(nst[:, slot, :], nst[:, slot, :], wC, PS[bd][:, 2:4], ALU.mult, ALU.add,
                        r=["nst", "sv", pk(bd)], w=["nst"])
                    free(bn, bi_, br, bc[0], bc[1])
                    tt(m_b, glb, nbl[:, bi, h:h + 1], ALU.subtract, r=["sv", ("nbl", bi)], w=["mst"])
                    act(t1[0:L, :], t2[0:L, :], AF.Square, accum=ss, r=["t2"], w=["t1", "sv"])
                    tt(vv, rden, rden, ALU.mult, r=["sv"], w=["sv"])
                    tt(vv, vv, ss, ALU.mult, r=["sv"], w=["sv"])
                    act(vv, vv, AF.Ln, bias=eps_ap[0:L], scale=1.0 / 512.0, r=["sv", "cf"], w=["sv"])
                    act(vv, vv, AF.Exp, scale=-0.5, r=["sv"], w=["sv"])
                    tt(sc, vv, rden, ALU.mult, r=["sv"], w=["sv"])
                    stt(t4[0:L, :], t2[0:L, :], sc, sig[0:L, hb, bi, :], ALU.mult, ALU.mult, r=["t2", "sv", sgk], w=["t4"])
                    if kind == "s":
                        dma("sp", oC[bi, h].rearrange("(j p) e -> p j e", p=128), Cst[:, slot], r=[Ck])
                        dma("sp", on[:, bi, h, :], nst[:, slot, :], r=["nst"])
                        dma("sp", om[:, bi, h:h + 1], mst[:, slot:slot + 1], r=["mst"])
                    elif last_prompt and bi == len(blocks) - 1:
                        dma("sp", pC[h].rearrange("(j p) e -> p j e", p=128), Cst[:, slot], r=[Ck])
                    yield
                    bt_ = bank()
                    pb = PS[bt_][:].bitcast(BF16)
                    for ec in range(4):
                        tr(pb[:, ec * L:(ec + 1) * L], t4[0:L, ec * 128:(ec + 1) * 128], identb[0:L, 0:L],
                           r=["t4", "cb"], w=[pk(bt_)], signal=(ec == 3))
                    for ec in range(4):
                        c = h * 4 + ec
                        ts(Dm[:, c, b0:b0 + L], pb[:, ec * L:(ec + 1) * L], ghead[:, c:c + 1], ALU.mult,
                           r=[pk(bt_), "vecs"], w=[Dk(c)])
                    free(bt_)
                    yield
                if last_prompt and h == 3:
                    dma("sp", pn, nst[:], r=["nst"])
                    dma("sp", pm, mst[:], r=["mst"])

            def do_head(h):
                hb = h % 2
                drain("a", 1)

                def q_cons(j, bk, hb=hb):
                    act(qT[:, hb, j, 0:ntok], PS[bk][:, 0:ntok], AF.Copy, scale=1.0 / 16.0, r=[pk(bk)], w=[("qT", hb, j)])
                ws_group(w_in, h * 256, 2, 32, u_rhs, u_keys, ntok, q_cons)

                def k_cons(j, bk, hb=hb):
                    cp(kT[:, hb, j, 0:ntok], PS[bk][:, 0:ntok], r=[pk(bk)], w=[("kT", hb, j)])
                ws_group(w_in, 1024 + h * 256, 2, 32, u_rhs, u_keys, ntok, k_cons)
                for bi, (b0, L) in enumerate(blocks):
                    bk = bank()
                    pb = PS[bk][:].bitcast(BF16)
                    for j in range(2):
                        tr(pb[0:L, j * 128:(j + 1) * 128], kT[:, hb, j, b0:b0 + L], identb, r=[("kT", hb, j), "cb"],
                           w=[pk(bk)], signal=(j == 1))
                    cp(ktok[0:L, hb, bi, :], pb[0:L, 0:256], r=[pk(bk)], w=[("ktok", hb, bi)])
                    free(bk)

                def v_cons(bi, bk, hb=hb):
                    L = blocks[bi][1]
                    cp(vtok[0:L, hb, bi, :], PS[bk][0:L, :], r=[pk(bk)], w=[("vtok", hb, bi)])
                as_group(w_in, 2048 + h * 512, blocks, v_cons)

                def o_cons(bi, bk, hb=hb):
                    L = blocks[bi][1]
                    act(sig[0:L, hb, bi, :], PS[bk][0:L, :], AF.Sigmoid, r=[pk(bk)], w=[("sig", hb, bi)])
                as_group(w_in, 4096 + h * 512, blocks, o_cons)
                lanes["a"].append(chunk_gen(h, hb))

            stage(6)
            def rg_gen(gp, pb_):
                for j in range(2):
                    gi = gp * 2 + j
                    xv = xrh[:, pb_, j, 0:nseg * (Lseg + 3)].rearrange("p (s l) -> p s l", s=nseg)
                    xk = ("xrh", pb_, j); gk_ = ("grb", pb_, j)
                    for s_, slot in enumerate(slots):
                        cp(xv[:, s_, 0:3], cvst[:, slot, gi, :], r=["cvst"], w=[xk])
                    xc3 = xc[:, 0:ntok].rearrange("p (s l) -> p s l", s=nseg)
                    ts(xc3, xv[:, :, 0:Lseg], cw[:, gi * 4:gi * 4 + 1], ALU.mult, convb[:, gi:gi + 1], ALU.add,
                       r=[xk, "vecs"], w=["xc"])
                    for tap in range(1, 4):
                        stt(xc3, xv[:, :, tap:tap + Lseg], cw[:, gi * 4 + tap:gi * 4 + tap + 1], xc3, ALU.mult, ALU.add,
                            r=[xk, "vecs", "xc"], w=["xc"])
                    for s_, slot in enumerate(slots):
                        cp(cvst[:, slot, gi, :], xv[:, s_, Lseg:Lseg + 3], r=[xk], w=["cvst"])
                    act(xcb[:, 0:ntok], xc[:, 0:ntok], AF.Copy, r=["xc"], w=["xcb"])
                    gx = grb[:, pb_, j, 0:ntok]
                    tt(rg_t[:, 0:ntok], gx, gx, ALU.mult, r=[gk_], w=["rg_t"])
                    ts(rg_t[:, 0:ntok], rg_t[:, 0:ntok], 0.044715, ALU.mult, 1.0, ALU.add, r=["rg_t"], w=["rg_t"])
                    tt(rg_t[:, 0:ntok], rg_t[:, 0:ntok], gx, ALU.mult, r=["rg_t", gk_], w=["rg_t"])
                    act(rg_t[:, 0:ntok], rg_t[:, 0:ntok], AF.Sigmoid, scale=1.5957691216057308, r=["rg_t"], w=["rg_t"])
                    tt(rg_t[:, 0:ntok], rg_t[:, 0:ntok], gx, ALU.mult, r=["rg_t", gk_], w=["rg_t"])
                    yield
                    b1 = bank(); b2 = bank()
                    mm(PS[b1][:, 0:ntok], wga[:, gi, :], xcb[:, 0:ntok], True, True, r=["xcb", "wga"], w=[pk(b1)])
                    mm(PS[b2][:, 0:ntok], wgx[:, gi, :], xcb[:, 0:ntok], True, True, r=["xcb", "wgx"], w=[pk(b2)])
                    act(rg_r[:, 0:ntok], PS[b1][:, 0:ntok], AF.Sigmoid, bias=bga[:, gi:gi + 1], r=[pk(b1), "vecs"], w=["rg_r"])
                    act(rg_i[:, 0:ntok], PS[b2][:, 0:ntok], AF.Sigmoid, bias=bgx[:, gi:gi + 1], r=[pk(b2), "vecs"], w=["rg_i"])
                    free(b1, b2)
                    act(rg_a[:, 0:ntok], rg_r[:, 0:ntok], AF.Exp, scale=nsp[:, gi:gi + 1], r=["rg_r", "nsp"], w=["rg_a"])
                    act(rg_m[:, 0:ntok], rg_r[:, 0:ntok], AF.Exp, scale=nsp[:, 16 + gi:17 + gi], r=["rg_r", "nsp"], w=["rg_m"])
                    ts(rg_m[:, 0:ntok], rg_m[:, 0:ntok], -1.0, ALU.mult, 1.0, ALU.add, r=["rg_m"], w=["rg_m"])
                    act(rg_m[:, 0:ntok], rg_m[:, 0:ntok], AF.Sqrt, r=["rg_m"], w=["rg_m"])
                    tt(rg_b[:, 0:ntok], rg_i[:, 0:ntok], xc[:, 0:ntok], ALU.mult, r=["rg_i", "xc"], w=["rg_b"])
                    if first_prompt:
                        cp(sv[:, 20:21], rg_b[:, 0:1], r=["rg_b"], w=["sv20"])
                    tt(rg_b[:, 0:ntok], rg_b[:, 0:ntok], rg_m[:, 0:ntok], ALU.mult, r=["rg_b", "rg_m"], w=["rg_b"])
                    if first_prompt:
                        cp(rg_b[:, 0:1], sv[:, 20:21], r=["sv20"], w=["rg_b"])
                    for s_, slot in enumerate(slots):
                        sl_ = slice(s_ * Lseg, (s_ + 1) * Lseg)
                        c0_ = s_ * Lseg
                        stt(rg_b[:, c0_:c0_ + 1], rg_a[:, c0_:c0_ + 1], hst[:, slot, gi:gi + 1], rg_b[:, c0_:c0_ + 1],
                            ALU.mult, ALU.add, r=["rg_a", "rg_b", "hst"], w=["rg_b"])
                        S.op("dve", lambda e, sl_=sl_: e.tensor_tensor_scan(
                            out=rg_h[:, sl_], data0=rg_a[:, sl_], data1=rg_b[:, sl_],
                            initial=0.0, op0=ALU.mult, op1=ALU.add),
                            ["rg_a", "rg_b"], ["rg_h"])
                        cp(hst[:, slot, gi:gi + 1], rg_h[:, (s_ + 1) * Lseg - 1:(s_ + 1) * Lseg], r=["rg_h"], w=["hst"])
                    tt(Dm[:, 16 + gi, 0:ntok], rg_t[:, 0:ntok], rg_h[:, 0:ntok], ALU.mult, r=["rg_t", "rg_h"], w=[Dk(16 + gi)])
                    yield
                if gp == 7:
                    if kind == "s":
                        dma("sp", oh, hst[:, 1:3, :], r=["hst"])
                        dma("sp", oconv, cvst[:, 1:3], r=["cvst"])
                    elif last_prompt:
                        dma("sp", ph, hst[:, 0, :], r=["hst"])
                        dma("sp", pconv, cvst[:, 0], r=["cvst"])

            def do_pair(gp):
                pb_ = gp % 2
                drain("b", 1)

                def xr_cons(j, bk, pb_=pb_):
                    act(xrh[:, pb_, j, 0:nseg * (Lseg + 3)].rearrange("p (s l) -> p s l", s=nseg)[:, :, 3:3 + Lseg],
                        PS[bk][:, 0:ntok].rearrange("p (s l) -> p s l", s=nseg), AF.Copy, r=[pk(bk)], w=[("xrh", pb_, j)])
                ws_group(w_in, 6152 + gp * 256, 2, 32, u_rhs, u_keys, ntok, xr_cons)

                def gr_cons(j, bk, pb_=pb_):
                    cp(grb[:, pb_, j, 0:ntok], PS[bk][:, 0:ntok], r=[pk(bk)], w=[("grb", pb_, j)])
                ws_group(w_in, 8200 + gp * 256, 2, 32, u_rhs, u_keys, ntok, gr_cons)
                lanes["b"].append(rg_gen(gp, pb_))

            for h in range(4):
                do_head(h)
                do_pair(2 * h)
                do_pair(2 * h + 1)
            drain()

            stage(7)
            d_rhs = lambda k: Dm[:, k, 0:ntok]
            d_keys = lambda k: [Dk(k)]
            for og in range(16):
                def mo_cons(j, bk, og=og):
                    c = og * 2 + j
                    if os.environ.get("MK_VAR") != "1":
                        cp(B[:, c, 0:ntok], PS[bk][:, 0:ntok], r=[pk(bk)], w=[Bk(c)])
                    if os.environ.get("MK_VAR") != "2":
                        accum_sq(c, PS[bk][:, 0:ntok], [pk(bk)], ntok)
                ws_group(w_out, og * 256, 2, 32, d_rhs, d_keys, ntok, mo_cons)
                stage(7.1)
            stage(7.2)
            finish_rstd(ntok, D)
            stage(7.3)
            for c in range(NCH):
                stt(B[:, c, 0:ntok], B[:, c, 0:ntok], gpm[:, c:c + 1], rstd[:, 0:ntok], ALU.mult, ALU.mult,
                    r=[Bk(c), "rstd", "vecs"], w=[Bk(c)])
                tt(A[:, c, 0:ntok], A[:, c, 0:ntok], B[:, c, 0:ntok], ALU.add, r=[Ak(c), Bk(c)], w=[Ak(c)])
            for c in range(NCH):
                accum_sq(c, A[:, c, 0:ntok], [Ak(c)], ntok)
            finish_rstd(ntok, D)
            for c in range(NCH):
                stt(U[:, c, 0:ntok], A[:, c, 0:ntok], gpf[:, c:c + 1], rstd[:, 0:ntok], ALU.mult, ALU.mult,
                    r=[Ak(c), "rstd", "vecs"], w=[Uk(c)])

            stage(8)
            parts = [(0, 15), (15, 14), (29, 14)]
            for pi, (g0, ng) in enumerate(parts):
                for gg in range(ng):
                    col = (g0 + gg) * 256
                    gb = []

                    def g_cons(j, bk):
                        gb.append(bk)
                    ws_group(w_g, col, 2, 32, u_rhs, u_keys, ntok, g_cons, hold=True)

                    def u_cons(j, bk, gg=gg):
                        gk = gb[j]
                        s = sqt[j]
                        act(s[:, 0:ntok], PS[gk][:, 0:ntok], AF.Silu, r=[pk(gk)], w=[("sqt", j)])
                        tt(Dm[:, gg * 2 + j, 0:ntok], s[:, 0:ntok], PS[bk][:, 0:ntok], ALU.mult,
                           r=[("sqt", j), pk(bk)], w=[Dk(gg * 2 + j)])
                        free(gk)
                    ws_group(w_u, col, 2, 32, u_rhs, u_keys, ntok, u_cons)
                K = ng * 2
                for og in range(8):
                    def dn_cons(j, bk, og=og, pi=pi):
                        c = og * 4 + j
                        if pi == 0:
                            cp(B[:, c, 0:ntok], PS[bk][:, 0:ntok], r=[pk(bk)], w=[Bk(c)])
                        else:
                            tt(B[:, c, 0:ntok], B[:, c, 0:ntok], PS[bk][:, 0:ntok], ALU.add, r=[Bk(c), pk(bk)], w=[Bk(c)])
                        if pi == 2:
                            accum_sq(c, B[:, c, 0:ntok], [Bk(c)], ntok)
                    ws_group(w_d[g0 * 256:(g0 + ng) * 256, :], og * 512, 4, K, d_rhs, d_keys, ntok, dn_cons)
            finish_rstd(ntok, D)
            for c in range(NCH):
                stt(B[:, c, 0:ntok], B[:, c, 0:ntok], gpo[:, c:c + 1], rstd[:, 0:ntok], ALU.mult, ALU.mult,
                    r=[Bk(c), "rstd", "vecs"], w=[Bk(c)])
                tt(B[:, c, 0:ntok], B[:, c, 0:ntok], A[:, c, 0:ntok], ALU.add, r=[Ak(c), Bk(c)], w=[Bk(c)])
            stage(9)
            for bi, (b0, L) in enumerate(blocks):
                for pc in range(4):
                    st = stg[pc % 2]; sk = ("stg", pc % 2)
                    for half in range(2):
                        bk = bank()
                        for j in range(4):
                            c = pc * 8 + half * 4 + j
                            tr(PS[bk][0:L, j * 128:(j + 1) * 128], B[:, c, b0:b0 + L], ident, r=[Bk(c), "cf"],
                               w=[pk(bk)], signal=(j == 3))
                        evac_copy(st[0:L, half * 512:(half + 1) * 512], PS[bk][0:L, :], r=[pk(bk)], w=[sk])
                        free(bk)
                    dma("sp", yd[row0 + b0:row0 + b0 + L, pc * 1024:(pc + 1) * 1024], st[0:L, :], r=[sk])

        try:
            do_tile(n_ptiles, "s")
            for ti in range(n_ptiles):
                do_tile(ti, "p")
        except StopBuild:
            pass
        S.finish()

        sems = {sk: nc.alloc_semaphore(name=f"s_{sk[0]}_{sk[1]}") for sk in sorted(S.semkeys, key=str)}
        engs = {"pe": "tensor", "act": "scalar", "dve": "vector", "pool": "gpsimd", "sp": "sync"}

        def replay(e, name):
            for waits, fn, sig_ in S.ops[name]:
                for sk, v in waits:
                    e.wait_ge(sems[sk], v)
                if fn is None:
                    continue
                ins = fn(e)
                if sig_ is not None:
                    ins.then_inc(sems[sig_[0]], sig_[1])

        with nc.Block() as block:
            @block.tensor
            def _(e):
                replay(e, "pe")

            @block.scalar
            def _(e):
                replay(e, "act")

            @block.vector
            def _(e):
                replay(e, "dve")

            @block.gpsimd
            def _(e):
                replay(e, "pool")

            @block.sync
            def _(e):
                replay(e, "sp")
    return nc


def _consts():
    idx = np.arange(128)
    ident = np.eye(128, dtype=np.float32)
    tri = (idx[:, None] <= idx[None, :]).astype(np.float32)
    maskA = np.where(idx[None, :] <= idx[:, None], 0.0, -BIG).astype(np.float32)
    maskB = np.where(idx[:, None] <= idx[None, :], 0.0, BIG).astype(np.float32)
    ones = np.ones((128, 128), np.float32)
    cst = np.zeros((128, 8), np.float32)
    cst[:, 0] = EPS
    cst[:, 1] = 1.0
    cf = np.concatenate([ident, tri, maskA, maskB, ones, cst], axis=1)
    cb = np.concatenate([ident, ones], axis=1).astype(ml_dtypes.bfloat16)
    return np.ascontiguousarray(cf), np.ascontiguousarray(cb)


def _fm(v, nchunks):
    return np.ascontiguousarray(np.asarray(v, np.float32).reshape(nchunks, 128).T)


_NC_CACHE = {}


def _run(inputs, n_cores, n_ptiles):
    f = lambda k: np.asarray(inputs[k], np.float32)
    xp_all = f("x_prompt"); xs_all = f("x_sample")
    SEQ = n_ptiles * NT
    assert xp_all.shape[1] == SEQ
    if n_ptiles not in _NC_CACHE:
        _NC_CACHE[n_ptiles] = build_nc(n_ptiles)
    nc = _NC_CACHE[n_ptiles]
    cf, cb = _consts()
    vecs = np.zeros((128, 352), np.float32)
    vecs[:, 0:32] = _fm(f("g_pre_mix")[0], 32); vecs[:, 32:64] = _fm(f("g_post_mix")[0], 32)
    vecs[:, 64:96] = _fm(f("g_pre_ffn")[0], 32); vecs[:, 96:128] = _fm(f("g_post_ffn")[0], 32)
    vecs[:, 128:144] = _fm(f("g_mlstm_head")[0], 16)
    cwv = f("conv_w")[0]
    vecs[:, 144:208] = cwv.reshape(4, 16, 128).transpose(2, 1, 0).reshape(128, 64)
    vecs[:, 208:224] = _fm(f("conv_b")[0], 16); vecs[:, 224:240] = _fm(f("b_rg_a")[0], 16)
    vecs[:, 240:256] = _fm(f("b_rg_x")[0], 16); vecs[:, 256:272] = _fm(f("rg_lambda")[0], 16)
    bif = np.ascontiguousarray(np.broadcast_to(
        np.concatenate([f("b_igate")[0], f("b_fgate")[0]])[None, :], (128, 8)))
    shared = {
        "w_in": f("w_in")[0], "w_out": f("w_out")[0], "w_g": f("w_ffn_gate")[0], "w_u": f("w_ffn_up")[0],
        "w_d": f("w_ffn_down")[0], "w_rga": f("w_rg_a")[0], "w_rgx": f("w_rg_x")[0],
        "vecs": vecs, "bif": bif, "cf": cf, "cb": cb,
    }
    sC = f("state_mlstm_C")[0]; sn = f("state_mlstm_n")[0]; sm = f("state_mlstm_m")[0]
    sh = f("state_rglru_h")[0]; scv = f("state_rglru_conv")[0]
    in_maps = []
    for c in range(n_cores):
        sl = slice(2 * c, 2 * c + 2)
        m = dict(shared)
        m["xp"] = np.ascontiguousarray(xp_all[c])
        m["xs"] = np.ascontiguousarray(xs_all[sl].reshape(64, D))
        m["sC"] = np.ascontiguousarray(sC[sl])
        m["sn"] = np.ascontiguousarray(sn[sl].reshape(2, 4, 2, 128).transpose(3, 0, 1, 2))
        m["sm"] = np.ascontiguousarray(np.broadcast_to(sm[sl][None], (128, 2, 4)))
        m["sh"] = np.ascontiguousarray(sh[sl].reshape(2, 16, 128).transpose(2, 0, 1))
        m["sconv"] = np.ascontiguousarray(scv[sl].reshape(2, 3, 16, 128).transpose(3, 0, 2, 1))
        in_maps.append(m)
    res = run_bass_kernel_spmd(nc, in_maps, core_ids=list(range(n_cores)))
    R = res.results
    B_, DB = n_cores, 2 * n_cores
    y_p = np.stack([R[c]["yp"] for c in range(B_)])
    y_s = np.concatenate([R[c]["ys"].reshape(2, 32, D) for c in range(B_)])
    p_C = np.stack([R[c]["pC"] for c in range(B_)])[None]
    p_n = np.stack([R[c]["pn"].transpose(1, 2, 0).reshape(4, 256) for c in range(B_)])[None]
    p_m = np.stack([R[c]["pm"][0] for c in range(B_)])[None]
    p_h = np.stack([R[c]["ph"].T.reshape(2048) for c in range(B_)])[None]
    p_conv = np.stack([R[c]["pconv"].transpose(2, 1, 0).reshape(3, 2048) for c in range(B_)])[None]
    s_C = np.concatenate([R[c]["oC"] for c in range(B_)])[None]
    s_n = np.concatenate([R[c]["on"].transpose(1, 2, 3, 0).reshape(2, 4, 256) for c in range(B_)])[None]
    s_m = np.concatenate([R[c]["om"][0] for c in range(B_)])[None]
    s_h = np.concatenate([R[c]["oh"].transpose(1, 2, 0).reshape(2, 2048) for c in range(B_)])[None]
    s_conv = np.concatenate([R[c]["oconv"].transpose(1, 3, 2, 0).reshape(2, 3, 2048) for c in range(B_)])[None]
    outs = (y_p, y_s, p_C, p_n, p_m, p_h, p_conv, s_C, s_n, s_m, s_h, s_conv)
    return tuple(np.ascontiguousarray(o, dtype=np.float32) for o in outs)


def kernel(**inputs):
    return _run(inputs, 8, 8)
```

```python
import contextlib
import os
import numpy as np
import ml_dtypes
import concourse.bass as bass
import concourse.mybir as mybir
from concourse.bass_utils import run_bass_kernel_spmd

F32 = mybir.dt.float32
BF16 = mybir.dt.bfloat16
AF = mybir.ActivationFunctionType
ALU = mybir.AluOpType
AX = mybir.AxisListType

D = 4096
NCH = 32
DFF = 11008
DIN = 10248
NT = 256
BIG = 30000.0
EPS = 1e-6
NWBUF = 4
COMPUTE = ("pe", "act", "dve", "pool")
MK_STAGE = float(os.environ.get("MK_STAGE", "99"))


class StopBuild(Exception):
    pass


def stage(n):
    if MK_STAGE < n:
        raise StopBuild()


class Sched:
    def __init__(self):
        self.ops = {e: [] for e in ("pe", "act", "dve", "pool", "sp")}
        self.cnt = {}
        self.epoch = 0
        self.waited = {e: {} for e in self.ops}
        self.lastw = {}
        self.readers = {}
        self.dma_pool = {"sp": [("dsp", i) for i in range(8)], "pool": [("dpl", i) for i in range(6)]}
        self.dma_rr = {"sp": 0, "pool": 0}
        self.dma_last = {}
        self.semkeys = set()

    def cur(self, eng):
        return (eng, self.epoch)

    def _deps(self, reads, writes):
        deps = []
        for k in reads:
            t = self.lastw.get(k)
            if t is not None:
                deps.append(t)
        for k in writes:
            t = self.lastw.get(k)
            if t is not None:
                deps.append(t)
            r = self.readers.get(k)
            if r:
                deps.extend(r.items())
        return deps

    def _filter(self, eng, deps):
        best = {}
        w = self.waited[eng]
        for sk, v in deps:
            if eng == "pe" and sk[0] == "pe":
                continue
            if w.get(sk, 0) >= v:
                continue
            if best.get(sk, 0) < v:
                best[sk] = v
        for sk, v in best.items():
            w[sk] = v
        return list(best.items())

    def _register(self, tok, reads, writes):
        sk, v = tok
        for k in reads:
            r = self.readers.setdefault(k, {})
            if r.get(sk, 0) < v:
                r[sk] = v
        for k in writes:
            self.lastw[k] = tok
            self.readers[k] = {}

    def op(self, eng, fn, reads=(), writes=(), signal=True):
        assert signal or eng == "pe"
        psr = [k for k in reads if isinstance(k, tuple) and k[0] == "ps"]
        if psr:
            reads = [k for k in reads if k not in psr]
            writes = list(writes) + psr
        waits = self._filter(eng, self._deps(reads, writes))
        sk = self.cur(eng)
        self.semkeys.add(sk)
        val = self.cnt.get(sk, 0) + 1
        if signal:
            self.cnt[sk] = val
        self._register((sk, val), reads, writes)
        self.ops[eng].append((waits, fn, (sk, 1) if signal else None))

    def dma(self, q, fn, reads=(), writes=()):
        deps = self._deps(reads, writes)
        pool = self.dma_pool[q]
        sk = pool[self.dma_rr[q] % len(pool)]
        self.dma_rr[q] += 1
        self.semkeys.add(sk)
        prev = self.dma_last.get(sk)
        if prev:
            deps.append((sk, prev))
        waits = self._filter(q, deps)
        val = self.cnt.get(sk, 0) + 16
        self.cnt[sk] = val
        self.dma_last[sk] = val
        self._register((sk, val), reads, writes)
        self.ops[q].append((waits, fn, (sk, 16)))

    def finish(self):
        waits = self._filter("sp", [(sk, v) for sk, v in self.dma_last.items()])
        self.ops["sp"].append((waits, None, None))


def build_nc(n_ptiles):
    SEQ = n_ptiles * NT
    nc = bass.Bass("TRN2", target_bir_lowering=False)
    S = Sched()

    def din(name, shape, dt=F32):
        return nc.dram_tensor(name, list(shape), dt, kind="ExternalInput").ap()

    def dout(name, shape, dt=F32):
        return nc.dram_tensor(name, list(shape), dt, kind="ExternalOutput").ap()

    xp = din("xp", [SEQ, D]); xs = din("xs", [64, D])
    sC_in = din("sC", [2, 4, 256, 512]); sn_in = din("sn", [128, 2, 4, 2]); sm_in = din("sm", [128, 2, 4])
    sh_in = din("sh", [128, 2, 16]); sconv_in = din("sconv", [128, 2, 16, 3])
    w_in = din("w_in", [D, DIN]); w_out = din("w_out", [D, D])
    w_g = din("w_g", [D, DFF]); w_u = din("w_u", [D, DFF]); w_d = din("w_d", [DFF, D])
    w_rga = din("w_rga", [16, 128, 128]); w_rgx = din("w_rgx", [16, 128, 128])
    vecs_d = din("vecs", [128, 352])
    bif_d = din("bif", [128, 8])
    cf_d = din("cf", [128, 5 * 128 + 8])
    cb_d = din("cb", [128, 256], BF16)

    yp = dout("yp", [SEQ, D]); ys = dout("ys", [64, D])
    pC = dout("pC", [4, 256, 512]); pn = dout("pn", [128, 4, 2]); pm = dout("pm", [128, 4])
    ph = dout("ph", [128, 16]); pconv = dout("pconv", [128, 16, 3])
    oC = dout("oC", [2, 4, 256, 512]); on = dout("on", [128, 2, 4, 2]); om = dout("om", [128, 2, 4])
    oh = dout("oh", [128, 2, 16]); oconv = dout("oconv", [128, 2, 16, 3])

    SCR_PER = 64
    scr = [nc.dram_tensor(f"wscr{i}", [SCR_PER, 128, 4096], BF16, kind="Internal").ap() for i in range(6)]

    es = contextlib.ExitStack()

    def sb(name, shape, dt=F32):
        return es.enter_context(nc.sbuf_tensor(name, list(shape), dt))

    with es:
        A = sb("A", [128, NCH, NT]); B = sb("B", [128, NCH, NT])
        U = sb("U", [128, NCH, NT], BF16); Dm = sb("Dm", [128, NCH, NT], BF16)
        W = [sb(f"W{i}", [128, 4096], BF16) for i in range(NWBUF)]
        Cst = sb("Cst", [128, 4, 2, 512]); nst = sb("nst", [128, 4, 2]); mst = sb("mst", [128, 4])
        hst = sb("hst", [128, 3, 16]); cvst = sb("cvst", [128, 3, 16, 3])
        wif = sb("wif", [128, 32, 8], BF16)
        wga = sb("wga", [128, 16, 128], BF16); wgx = sb("wgx", [128, 16, 128], BF16)
        vecs = sb("vecs_s", [128, 352]); bif = sb("bif_s", [128, 8])
        cf = sb("cf_s", [128, 5 * 128 + 8]); cb = sb("cb_s", [128, 256], BF16)
        nsp = sb("nsp", [128, 32])
        stg = [sb(f"stg{i}", [128, 512]) for i in range(2)]
        acc = sb("acc", [128, NT]); sqt = [sb(f"sqt{i}", [128, NT]) for i in range(2)]
        rstd = sb("rstd", [128, NT]); lnv = sb("lnv", [128, NT])
        qT = sb("qT", [128, 2, 2, NT], BF16); kT = sb("kT", [128, 2, 2, NT], BF16)
        ktok = sb("ktok", [128, 2, 2, 256], BF16); vtok = sb("vtok", [128, 2, 2, 512], BF16)
        sig = sb("sig", [128, 2, 2, 512], BF16)
        igf = sb("igf", [128, 2, 8]); sp8 = sb("sp8", [128, 2, 8]); cvec = sb("cvec", [128, 2, 4])
        nb = sb("nb", [128, 2, 4]); nbl = sb("nbl", [128, 2, 4])
        dg = sb("dg", [128, 128]); dg2 = sb("dg2", [128, 128]); tll = sb("tll", [128, 128])
        dmT = sb("dmT", [128, 128]); PT = sb("PT", [128, 128], BF16)
        t1 = sb("t1", [128, 512]); t2 = sb("t2", [128, 512]); t4 = sb("t4", [128, 512], BF16)
        kw = sb("kw", [128, 256], BF16); Cbf = sb("Cbf", [128, 2, 512], BF16); nbf = sb("nbf", [128, 2], BF16)
        sv = sb("sv", [128, 24])
        xrh = sb("xrh", [128, 2, 2, NT + 8]); grb = sb("grb", [128, 2, 2, NT])
        xc = sb("xc", [128, NT]); xcb = sb("xcb", [128, NT], BF16)
        rg_r = sb("rg_r", [128, NT]); rg_i = sb("rg_i", [128, NT]); rg_a = sb("rg_a", [128, NT])
        rg_m = sb("rg_m", [128, NT]); rg_b = sb("rg_b", [128, NT]); rg_h = sb("rg_h", [128, NT])
        rg_t = sb("rg_t", [128, NT])
        PS = [es.enter_context(nc.psum_tensor(f"ps{i}", [128, 512], F32)) for i in range(8)]

        ident = cf[:, 0:128]; tri = cf[:, 128:256]; maskA = cf[:, 256:384]; maskB = cf[:, 384:512]
        ones = cf[:, 512:640]; cst = cf[:, 640:648]
        eps_ap = cst[:, 0:1]; one_ap = cst[:, 1:2]
        identb = cb[:, 0:128]; onesb = cb[:, 128:256]
        gpre = vecs[:, 0:32]; gpm = vecs[:, 32:64]; gpf = vecs[:, 64:96]; gpo = vecs[:, 96:128]
        ghead = vecs[:, 128:144]; cw = vecs[:, 144:208]; convb = vecs[:, 208:224]
        bga = vecs[:, 224:240]; bgx = vecs[:, 240:256]; lam = vecs[:, 256:272]

        free_banks = list(range(8))

        def bank():
            assert free_banks, "PSUM banks exhausted"
            return free_banks.pop(0)

        def free(*bs):
            for b in bs:
                assert b not in free_banks
                free_banks.append(b)

        def pk(i):
            return ("ps", i)

        def act(out, in_, func, bias=None, scale=None, accum=None, r=(), w=()):
            def fn(e):
                kw_ = {}
                if bias is not None:
                    kw_["bias"] = bias
                if scale is not None:
                    kw_["scale"] = scale
                if accum is not None:
                    kw_["accum_out"] = accum
                return e.activation(out=out, in_=in_, func=func, **kw_)
            S.op("act", fn, r, w)

        def tt(out, in0, in1, op, r=(), w=(), eng="dve"):
            S.op(eng, lambda e: e.tensor_tensor(out=out, in0=in0, in1=in1, op=op), r, w)

        def ts(out, in0, s1, op0, s2=None, op1=None, r=(), w=(), eng="dve"):
            def fn(e):
                if op1 is None:
                    return e.tensor_scalar(out=out, in0=in0, scalar1=s1, scalar2=None, op0=op0)
                return e.tensor_scalar(out=out, in0=in0, scalar1=s1, scalar2=s2, op0=op0, op1=op1)
            S.op(eng, fn, r, w)

        def stt(out, in0, sc, in1, op0, op1, r=(), w=()):
            S.op("dve", lambda e: e.scalar_tensor_tensor(out=out, in0=in0, scalar=sc, in1=in1, op0=op0, op1=op1), r, w)

        def cp(out, in_, r=(), w=(), eng="dve"):
            S.op(eng, lambda e: e.tensor_copy(out=out, in_=in_), r, w)

        def mm(out, lhsT, rhs, start, stop, r=(), w=(), signal=None):
            S.op("pe", lambda e: e.matmul(out, lhsT=lhsT, rhs=rhs, start=start, stop=stop), r, w,
                 signal=stop if signal is None else signal)

        def tr(out, in_, idn, r=(), w=(), signal=True):
            S.op("pe", lambda e: e.transpose(out=out, in_=in_, identity=idn), r, w, signal=signal)

        def dma(q, out, in_, r=(), w=(), nonc=False):
            def fn(e):
                with nc.allow_non_contiguous_dma(reason="small"):
                    return e.dma_start(out=out, in_=in_)
            S.dma(q, fn, r, w)

        evrr = [0]

        def evac_copy(out, in_, r, w):
            evrr[0] += 1
            if evrr[0] % 2:
                cp(out, in_, r, w)
            else:
                act(out, in_, AF.Copy, r=r, w=w)

        lanes = {"a": [], "b": []}

        def tick1(q):
            while q:
                try:
                    next(q[0])
                    return
                except StopIteration:
                    q.pop(0)

        tick_n = [0]

        def tick():
            tick_n[0] += 1
            if tick_n[0] % 2:
                return
            tick1(lanes["a"])
            tick1(lanes["b"])

        def drain(lane=None, maxlen=0):
            for nm in ([lane] if lane else ["a", "b"]):
                q = lanes[nm]
                while len(q) > maxlen:
                    tick1(q)
                    if lane is None or True:
                        other = lanes["b" if nm == "a" else "a"]
                        tick1(other)

        wrr = [0]

        slab_idx = [0]
        slab_mode = ["cast"]

        def slab(wd, r0, nk, c0, ncol):
            i = wrr[0] % NWBUF
            wrr[0] += 1
            si = slab_idx[0]
            slab_idx[0] += 1
            flat = W[i][:, 0:nk * ncol]
            view = flat.rearrange("p (k c) -> p k c", k=nk)
            sdst = scr[si // SCR_PER][si % SCR_PER][:, 0:nk * ncol]
            if slab_mode[0] == "cast":
                src = wd[r0:r0 + nk * 128, c0:c0 + ncol].rearrange("(k p) c -> p k c", p=128)
                dma("pool", view, src, w=[("W", i)])
                dma("sp", sdst, flat, r=[("W", i)], w=[("scr", si)])
            else:
                dma("pool", flat, sdst, r=[("scr", si)], w=[("W", i)])
            return view, ("W", i)

        def ws_group(wd, c0, nch, K, rhs_of, rkeys_of, ncols, consume, hold=False):
            ncol = nch * 128
            nk_max = 4096 // ncol
            banks = [bank() for _ in range(nch)]
            k0 = 0
            while k0 < K:
                nk = min(nk_max, K - k0)
                view, wkey = slab(wd, k0 * 128, nk, c0, ncol)
                for j in range(nch):
                    for kk in range(nk):
                        k = k0 + kk
                        last_in_slab = (j == nch - 1 and kk == nk - 1)
                        mm(PS[banks[j]][:, 0:ncols], view[:, kk, j * 128:(j + 1) * 128], rhs_of(k),
                           start=(k == 0), stop=(k == K - 1), r=[wkey] + rkeys_of(k), w=[pk(banks[j])],
                           signal=(k == K - 1) or last_in_slab)
                k0 += nk
                tick()
            for j in range(nch):
                consume(j, banks[j])
                if not hold:
                    free(banks[j])

        def as_group(wd, c0, blocks, consume):
            banks = [bank() for _ in blocks]
            for k0 in range(0, 32, 8):
                view, wkey = slab(wd, k0 * 128, 8, c0, 512)
                for bi, (b0, L) in enumerate(blocks):
                    for kk in range(8):
                        k = k0 + kk
                        last_in_slab = (bi == len(blocks) - 1 and kk == 7)
                        mm(PS[banks[bi]][0:L, :], U[:, k, b0:b0 + L], view[:, kk, :],
                           start=(k == 0), stop=(k == 31), r=[wkey, ("U", k)], w=[pk(banks[bi])],
                           signal=(k == 31) or last_in_slab)
                tick()
            for bi in range(len(blocks)):
                consume(bi, banks[bi])
                free(banks[bi])

        def finish_rstd(ntok, dim):
            bi = bank()
            mm(PS[bi][:, 0:ntok], ones, acc[:, 0:ntok], True, True, r=["acc"], w=[pk(bi)])
            act(lnv[:, 0:ntok], PS[bi][:, 0:ntok], AF.Ln, bias=eps_ap, scale=1.0 / dim, r=[pk(bi)], w=["lnv"])
            act(rstd[:, 0:ntok], lnv[:, 0:ntok], AF.Exp, scale=-0.5, r=["lnv"], w=["rstd"])
            free(bi)

        def accum_sq(c, src, srckeys, ntok):
            if c == 0:
                act(acc[:, 0:ntok], src, AF.Square, r=srckeys, w=["acc"])
            else:
                s = sqt[c % 2]
                act(s[:, 0:ntok], src, AF.Square, r=srckeys, w=[("sqt", c % 2)])
                tt(acc[:, 0:ntok], acc[:, 0:ntok], s[:, 0:ntok], ALU.add, r=[("sqt", c % 2), "acc"], w=["acc"])

        dma("sp", cf[:], cf_d, w=["cf"]); dma("sp", cb[:], cb_d, w=["cb"])
        dma("sp", vecs[:], vecs_d, w=["vecs"]); dma("sp", bif[:], bif_d, w=["bif"])
        dma("pool", wif[:], w_in[:, 6144:6152].rearrange("(k p) c -> p k c", p=128), w=["wif"], nonc=True)
        dma("pool", wga[:], w_rga.rearrange("g i j -> i g j"), w=["wga"])
        dma("pool", wgx[:], w_rgx.rearrange("g i j -> i g j"), w=["wgx"])
        act(nsp[:, 0:16], lam, AF.Exp, scale=-1.0, r=["vecs"], w=["nsp"])
        act(nsp[:, 16:32], nsp[:, 0:16], AF.Ln, bias=one_ap, r=["nsp", "cf"], w=["nsp"])
        ts(nsp[:, 0:16], nsp[:, 16:32], -8.0, ALU.mult, r=["nsp"], w=["nsp"])
        ts(nsp[:, 16:32], nsp[:, 16:32], -16.0, ALU.mult, r=["nsp"], w=["nsp"])
        S.op("dve", lambda e: e.memset(hst[:, 0, :], 0.0), (), ["hst"])
        S.op("dve", lambda e: e.memset(cvst[:, 0].rearrange("p a b -> p (a b)"), 0.0), (), ["cvst"])
        dma("sp", hst[:, 1:3, :], sh_in, w=["hst"])
        dma("sp", cvst[:, 1:3], sconv_in, w=["cvst"])
        const_keys = ["cf", "cb", "vecs", "bif", "wif", "wga", "wgx", "nsp"]

        def do_tile(ti, kind):
            S.epoch = ti + 1 if kind == "p" else 0
            slab_idx[0] = 0
            slab_mode[0] = "cast" if kind == "s" else "scr"
            if kind == "p" and ti == 0:
                S.op("dve", lambda e: e.memset(Cst[:].rearrange("p a b c -> p (a b c)"), 0.0), (), [("Cst", h) for h in range(4)])
                S.op("dve", lambda e: e.memset(nst[:].rearrange("p a b -> p (a b)"), 0.0), (), ["nst"])
                S.op("dve", lambda e: e.memset(mst[:], 0.0), (), ["mst"])
            if kind == "p":
                xd, yd, row0 = xp, yp, ti * NT
                blocks = [(0, 128), (128, 128)]
                nseg, Lseg, ntok = 1, NT, NT
                slots = [0]
            else:
                xd, yd, row0 = xs, ys, 0
                blocks = [(0, 32), (32, 32)]
                nseg, Lseg, ntok = 2, 32, 64
                slots = [1, 2]
            first_prompt = (kind == "p" and ti == 0)
            last_prompt = (kind == "p" and ti == n_ptiles - 1)
            Ak = lambda c: ("A", c)
            Bk = lambda c: ("B", c)
            Uk = lambda c: ("U", c)
            Dk = lambda c: ("D", c)

            for bi, (b0, L) in enumerate(blocks):
                for pc in range(8):
                    st = stg[pc % 2]; sk = ("stg", pc % 2)
                    dma("sp", st[0:L, :], xd[row0 + b0:row0 + b0 + L, pc * 512:(pc + 1) * 512], w=[sk])
                    bk = bank()
                    for j in range(4):
                        tr(PS[bk][:, j * L:(j + 1) * L], st[0:L, j * 128:(j + 1) * 128],
                           ident[0:L, 0:L], r=[sk, "cf"], w=[pk(bk)], signal=(j == 3))
                    c0 = pc * 4
                    evac_copy(A[:, c0:c0 + 4, b0:b0 + L], PS[bk][:, 0:4 * L].rearrange("p (j l) -> p j l", j=4),
                              r=[pk(bk)], w=[Ak(c0 + j) for j in range(4)])
                    free(bk)
            stage(1)
            for c in range(NCH):
                accum_sq(c, A[:, c, 0:ntok], [Ak(c)], ntok)
            finish_rstd(ntok, D)
            for c in range(NCH):
                stt(U[:, c, 0:ntok], A[:, c, 0:ntok], gpre[:, c:c + 1], rstd[:, 0:ntok], ALU.mult, ALU.mult,
                    r=[Ak(c), "rstd", "vecs"], w=[Uk(c)])

            u_rhs = lambda k: U[:, k, 0:ntok]
            u_keys = lambda k: [Uk(k)]

            stage(2)
            for bi, (b0, L) in enumerate(blocks):
                bk = bank()
                for k in range(32):
                    mm(PS[bk][0:L, 0:8], U[:, k, b0:b0 + L], wif[:, k, :], k == 0, k == 31,
                       r=[Uk(k), "wif"], w=[pk(bk)])
                tt(igf[0:L, bi, :], PS[bk][0:L, 0:8], bif[0:L, :], ALU.add, r=[pk(bk), "bif"], w=[("igf", bi)])
                free(bk)
                act(sp8[0:L, bi, 0:4], igf[0:L, bi, 4:8], AF.Exp, scale=-1.0, r=[("igf", bi)], w=[("sp8", bi)])
                act(sp8[0:L, bi, 4:8], sp8[0:L, bi, 0:4], AF.Ln, bias=one_ap[0:L], r=[("sp8", bi), "cf"], w=[("sp8", bi)])
                bk2 = bank()
                mm(PS[bk2][0:L, 0:4], tri[0:L, 0:L], sp8[0:L, bi, 4:8], True, True, r=[("sp8", bi), "cf"], w=[pk(bk2)])
                mm(PS[bk2][:, 4:8], ones[0:L, :], sp8[0:L, bi, 4:8], True, True, r=[("sp8", bi), "cf"], w=[pk(bk2)])
                cp(nb[0:L, bi, :], PS[bk2][0:L, 0:4], r=[pk(bk2)], w=[("nb", bi)])
                cp(nbl[:, bi, :], PS[bk2][:, 4:8], r=[pk(bk2)], w=[("nbl", bi)])
                free(bk2)
                tt(cvec[0:L, bi, :], igf[0:L, bi, 0:4], nb[0:L, bi, :], ALU.add, r=[("igf", bi), ("nb", bi)], w=[("cvec", bi)])

            stage(3)
            def chunk_gen(h, hb):
                for bi, (b0, L) in enumerate(blocks):
                    if kind == "p":
                        slot = h
                    else:
                        slot = (2 * h + bi) % 4
                        dma("sp", Cst[:, slot], sC_in[bi, h].rearrange("(j p) e -> p j e", p=128), w=[("Cst", slot)])
                        dma("sp", nst[:, slot, :], sn_in[:, bi, h, :], w=["nst"])
                        dma("sp", mst[:, slot:slot + 1], sm_in[:, bi, h:h + 1], w=["mst"])
                    Ck = ("Cst", slot)
                    qk = lambda j: ("qT", hb, j)
                    kk_ = lambda j: ("kT", hb, j)
                    vk = ("vtok", hb, bi); sgk = ("sig", hb, bi); ktk = ("ktok", hb, bi)
                    c_h = cvec[0:L, bi, h:h + 1]
                    m_b = mst[:, slot:slot + 1]
                    cm = sv[0:L, 0:1]; g = sv[0:L, 1:2]; glb = sv[:, 2:3]; nglb = sv[:, 3:4]
                    winter = sv[0:L, 4:5]; r2 = sv[0:L, 5:7]; den = sv[0:L, 7:8]; mj = sv[0:L, 8:9]
                    emj = sv[0:L, 9:10]; rden = sv[0:L, 10:11]; ss = sv[0:L, 11:12]; vv = sv[0:L, 12:13]
                    sc = sv[0:L, 13:14]; wsv = sv[0:L, 14:15]; wC = sv[:, 15:16]
                    bs = bank()
                    for j in range(2):
                        mm(PS[bs][0:L, 0:L], kT[:, hb, j, b0:b0 + L], qT[:, hb, j, b0:b0 + L], j == 0, j == 1,
                           r=[kk_(j), qk(j)], w=[pk(bs)])
                    cp(Cbf[:].rearrange("p a b -> p (a b)"), Cst[:, slot].rearrange("p a b -> p (a b)"), r=[Ck], w=["Cbf"])
                    cp(nbf[:], nst[:, slot, :], r=["nst"], w=["nbf"])
                    ts(dg[0:L, 0:L], ident[0:L, 0:L], c_h, ALU.mult, r=[("cvec", bi), "cf"], w=["dg"])
                    yield
                    bx = bank()
                    mm(PS[bx][0:L, 0:L], ones[0:L, 0:L], dg[0:L, 0:L], True, True, r=["dg", "cf"], w=[pk(bx)])
                    tt(tll[0:L, 0:L], PS[bx][0:L, 0:L], maskA[0:L, 0:L], ALU.add, r=[pk(bx), "cf"], w=["tll"])
                    free(bx)
                    S.op("dve", lambda e, cm=cm, L=L: e.reduce_max(out=cm, in_=tll[0:L, 0:L], axis=AX.X), ["tll"], ["sv"])
                    tt(g, cm, m_b[0:L], ALU.max, r=["sv", "mst"], w=["sv"])
                    ts(dg2[0:L, 0:L], ident[0:L, 0:L], g, ALU.mult, r=["sv", "cf"], w=["dg2"])
                    yield
                    by = bank()
                    mm(PS[by][:, 0:L], ones[0:L, :], dg2[0:L, 0:L], True, True, r=["dg2", "cf"], w=[pk(by)])
                    tt(tll[0:L, 0:L], PS[by][0:L, 0:L], maskB[0:L, 0:L], ALU.add, r=[pk(by), "cf"], w=["tll"])
                    cp(glb, PS[by][:, L - 1:L], r=[pk(by)], w=["sv"])
                    free(by)
                    act(dmT[0:L, 0:L], tll[0:L, 0:L], AF.Exp, bias=c_h, scale=-1.0, r=["tll", ("cvec", bi)], w=["dmT"])
                    ts(nglb, glb, -1.0, ALU.mult, r=["sv"], w=["sv"])
                    tt(PT[0:L, 0:L], PS[bs][0:L, 0:L], dmT[0:L, 0:L], ALU.mult, r=[pk(bs), "dmT"], w=["PT"])
                    free(bs)
                    act(winter, g, AF.Exp, bias=m_b[0:L], scale=-1.0, r=["sv", "mst"], w=["sv"])
                    tt(mj, g, nb[0:L, bi, h:h + 1], ALU.subtract, r=["sv", ("nb", bi)], w=["sv"])
                    act(emj, mj, AF.Exp, scale=-1.0, r=["sv"], w=["sv"])
                    act(wsv, c_h, AF.Exp, bias=nglb[0:L], r=[("cvec", bi), "sv"], w=["sv"])
                    act(wC, m_b, AF.Exp, bias=nglb, r=["mst", "sv"], w=["sv"])
                    ts(kw[0:L, :], ktok[0:L, hb, bi, :], wsv, ALU.mult, r=[ktk, "sv"], w=["kw"])
                    yield
                    bn = bank()
                    mm(PS[bn][0:L, :], PT[0:L, 0:L], vtok[0:L, hb, bi, :], True, True, r=["PT", vk], w=[pk(bn)])
                    bi_ = bank()
                    for j in range(2):
                        mm(PS[bi_][0:L, :], qT[:, hb, j, b0:b0 + L], Cbf[:, j, :], j == 0, j == 1,
                           r=[qk(j), "Cbf"], w=[pk(bi_)])
                    br = bank()
                    mm(PS[br][0:L, 0:1], PT[0:L, 0:L], onesb[0:L, 0:1], True, True, r=["PT", "cb"], w=[pk(br)])
                    for j in range(2):
                        mm(PS[br][0:L, 1:2], qT[:, hb, j, b0:b0 + L], nbf[:, j:j + 1], j == 0, j == 1,
                           r=[qk(j), "nbf"], w=[pk(br)])
                    bc = [bank(), bank()]
                    for j in range(2):
                        mm(PS[bc[j]][:, :], kw[0:L, j * 128:(j + 1) * 128], vtok[0:L, hb, bi, :], True, True,
                           r=["kw", vk], w=[pk(bc[j])])
                    bd = br
                    for j in range(2):
                        mm(PS[bd][:, 2 + j:3 + j], kw[0:L, j * 128:(j + 1) * 128], onesb[0:L, 0:1], True, True,
                           r=["kw", "cb"], w=[pk(bd)])
                    cp(r2, PS[br][0:L, 0:2], r=[pk(br)], w=["sv"])
                    stt(den, r2[:, 1:2], winter, r2[:, 0:1], ALU.mult, ALU.add, r=["sv"], w=["sv"])
                    stt(den, den, -1.0, den, ALU.mult, ALU.max, r=["sv"], w=["sv"])
                    tt(den, den, emj, ALU.max, r=["sv"], w=["sv"])
                    S.op("dve", lambda e, rden=rden, den=den: e.reciprocal(out=rden, in_=den), ["sv"], ["sv"])
                    act(t1[0:L, :], PS[bi_][0:L, :], AF.Copy, scale=winter, r=[pk(bi_), "sv"], w=["t1"])
                    tt(t2[0:L, :], PS[bn][0:L, :], t1[0:L, :], ALU.add, r=[pk(bn), "t1"], w=["t2"])
                    for j in range(2):
                        stt(Cst[:, slot, j, :], Cst[:, slot, j, :], wC, PS[bc[j]][:, :], ALU.mult, ALU.add,
                            r=[Ck, "sv", pk(bc[j])], w=[Ck])
                    stt(nst[:, slot, :], nst[:, slot, :], wC, PS[bd][:, 2:4], ALU.mult, ALU.add,
                        r=["nst", "sv", pk(bd)], w=["nst"])
                    free(bn, bi_, br, bc[0], bc[1])
                    tt(m_b, glb, nbl[:, bi, h:h + 1], ALU.subtract, r=["sv", ("nbl", bi)], w=["mst"])
                    act(t1[0:L, :], t2[0:L, :], AF.Square, accum=ss, r=["t2"], w=["t1", "sv"])
                    tt(vv, rden, rden, ALU.mult, r=["sv"], w=["sv"])
                    tt(vv, vv, ss, ALU.mult, r=["sv"], w=["sv"])
                    act(vv, vv, AF.Ln, bias=eps_ap[0:L], scale=1.0 / 512.0, r=["sv", "cf"], w=["sv"])
                    act(vv, vv, AF.Exp, scale=-0.5, r=["sv"], w=["sv"])
                    tt(sc, vv, rden, ALU.mult, r=["sv"], w=["sv"])
                    stt(t4[0:L, :], t2[0:L, :], sc, sig[0:L, hb, bi, :], ALU.mult, ALU.mult, r=["t2", "sv", sgk], w=["t4"])
                    if kind == "s":
                        dma("sp", oC[bi, h].rearrange("(j p) e -> p j e", p=128), Cst[:, slot], r=[Ck])
                        dma("sp", on[:, bi, h, :], nst[:, slot, :], r=["nst"])
                        dma("sp", om[:, bi, h:h + 1], mst[:, slot:slot + 1], r=["mst"])
                    elif last_prompt and bi == len(blocks) - 1:
                        dma("sp", pC[h].rearrange("(j p) e -> p j e", p=128), Cst[:, slot], r=[Ck])
                    yield
                    bt_ = bank()
                    pb = PS[bt_][:].bitcast(BF16)
                    for ec in range(4):
                        tr(pb[:, ec * L:(ec + 1) * L], t4[0:L, ec * 128:(ec + 1) * 128], identb[0:L, 0:L],
                           r=["t4", "cb"], w=[pk(bt_)], signal=(ec == 3))
                    for ec in range(4):
                        c = h * 4 + ec
                        ts(Dm[:, c, b0:b0 + L], pb[:, ec * L:(ec + 1) * L], ghead[:, c:c + 1], ALU.mult,
                           r=[pk(bt_), "vecs"], w=[Dk(c)])
                    free(bt_)
                    yield
                if last_prompt and h == 3:
                    dma("sp", pn, nst[:], r=["nst"])
                    dma("sp", pm, mst[:], r=["mst"])

            def do_head(h):
                hb = h % 2
                drain("a", 1)

                def q_cons(j, bk, hb=hb):
                    act(qT[:, hb, j, 0:ntok], PS[bk][:, 0:ntok], AF.Copy, scale=1.0 / 16.0, r=[pk(bk)], w=[("qT", hb, j)])
                ws_group(w_in, h * 256, 2, 32, u_rhs, u_keys, ntok, q_cons)

                def k_cons(j, bk, hb=hb):
                    cp(kT[:, hb, j, 0:ntok], PS[bk][:, 0:ntok], r=[pk(bk)], w=[("kT", hb, j)])
                ws_group(w_in, 1024 + h * 256, 2, 32, u_rhs, u_keys, ntok, k_cons)
                for bi, (b0, L) in enumerate(blocks):
                    bk = bank()
                    pb = PS[bk][:].bitcast(BF16)
                    for j in range(2):
                        tr(pb[0:L, j * 128:(j + 1) * 128], kT[:, hb, j, b0:b0 + L], identb, r=[("kT", hb, j), "cb"],
                           w=[pk(bk)], signal=(j == 1))
                    cp(ktok[0:L, hb, bi, :], pb[0:L, 0:256], r=[pk(bk)], w=[("ktok", hb, bi)])
                    free(bk)

                def v_cons(bi, bk, hb=hb):
                    L = blocks[bi][1]
                    cp(vtok[0:L, hb, bi, :], PS[bk][0:L, :], r=[pk(bk)], w=[("vtok", hb, bi)])
                as_group(w_in, 2048 + h * 512, blocks, v_cons)

                def o_cons(bi, bk, hb=hb):
                    L = blocks[bi][1]
                    act(sig[0:L, hb, bi, :], PS[bk][0:L, :], AF.Sigmoid, r=[pk(bk)], w=[("sig", hb, bi)])
                as_group(w_in, 4096 + h * 512, blocks, o_cons)
                lanes["a"].append(chunk_gen(h, hb))

            stage(6)
            def rg_gen(gp, pb_):
                for j in range(2):
                    gi = gp * 2 + j
                    xv = xrh[:, pb_, j, 0:nseg * (Lseg + 3)].rearrange("p (s l) -> p s l", s=nseg)
                    xk = ("xrh", pb_, j); gk_ = ("grb", pb_, j)
                    for s_, slot in enumerate(slots):
                        cp(xv[:, s_, 0:3], cvst[:, slot, gi, :], r=["cvst"], w=[xk])
                    xc3 = xc[:, 0:ntok].rearrange("p (s l) -> p s l", s=nseg)
                    ts(xc3, xv[:, :, 0:Lseg], cw[:, gi * 4:gi * 4 + 1], ALU.mult, convb[:, gi:gi + 1], ALU.add,
                       r=[xk, "vecs"], w=["xc"])
                    for tap in range(1, 4):
                        stt(xc3, xv[:, :, tap:tap + Lseg], cw[:, gi * 4 + tap:gi * 4 + tap + 1], xc3, ALU.mult, ALU.add,
                            r=[xk, "vecs", "xc"], w=["xc"])
                    for s_, slot in enumerate(slots):
                        cp(cvst[:, slot, gi, :], xv[:, s_, Lseg:Lseg + 3], r=[xk], w=["cvst"])
                    act(xcb[:, 0:ntok], xc[:, 0:ntok], AF.Copy, r=["xc"], w=["xcb"])
                    gx = grb[:, pb_, j, 0:ntok]
                    tt(rg_t[:, 0:ntok], gx, gx, ALU.mult, r=[gk_], w=["rg_t"])
                    ts(rg_t[:, 0:ntok], rg_t[:, 0:ntok], 0.044715, ALU.mult, 1.0, ALU.add, r=["rg_t"], w=["rg_t"])
                    tt(rg_t[:, 0:ntok], rg_t[:, 0:ntok], gx, ALU.mult, r=["rg_t", gk_], w=["rg_t"])
                    act(rg_t[:, 0:ntok], rg_t[:, 0:ntok], AF.Sigmoid, scale=1.5957691216057308, r=["rg_t"], w=["rg_t"])
                    tt(rg_t[:, 0:ntok], rg_t[:, 0:ntok], gx, ALU.mult, r=["rg_t", gk_], w=["rg_t"])
                    yield
                    b1 = bank(); b2 = bank()
                    mm(PS[b1][:, 0:ntok], wga[:, gi, :], xcb[:, 0:ntok], True, True, r=["xcb", "wga"], w=[pk(b1)])
                    mm(PS[b2][:, 0:ntok], wgx[:, gi, :], xcb[:, 0:ntok], True, True, r=["xcb", "wgx"], w=[pk(b2)])
                    act(rg_r[:, 0:ntok], PS[b1][:, 0:ntok], AF.Sigmoid, bias=bga[:, gi:gi + 1], r=[pk(b1), "vecs"], w=["rg_r"])
                    act(rg_i[:, 0:ntok], PS[b2][:, 0:ntok], AF.Sigmoid, bias=bgx[:, gi:gi + 1], r=[pk(b2), "vecs"], w=["rg_i"])
                    free(b1, b2)
                    act(rg_a[:, 0:ntok], rg_r[:, 0:ntok], AF.Exp, scale=nsp[:, gi:gi + 1], r=["rg_r", "nsp"], w=["rg_a"])
                    act(rg_m[:, 0:ntok], rg_r[:, 0:ntok], AF.Exp, scale=nsp[:, 16 + gi:17 + gi], r=["rg_r", "nsp"], w=["rg_m"])
                    ts(rg_m[:, 0:ntok], rg_m[:, 0:ntok], -1.0, ALU.mult, 1.0, ALU.add, r=["rg_m"], w=["rg_m"])
                    act(rg_m[:, 0:ntok], rg_m[:, 0:ntok], AF.Sqrt, r=["rg_m"], w=["rg_m"])
                    tt(rg_b[:, 0:ntok], rg_i[:, 0:ntok], xc[:, 0:ntok], ALU.mult, r=["rg_i", "xc"], w=["rg_b"])
                    if first_prompt:
                        cp(sv[:, 20:21], rg_b[:, 0:1], r=["rg_b"], w=["sv20"])
                    tt(rg_b[:, 0:ntok], rg_b[:, 0:ntok], rg_m[:, 0:ntok], ALU.mult, r=["rg_b", "rg_m"], w=["rg_b"])
                    if first_prompt:
                        cp(rg_b[:, 0:1], sv[:, 20:21], r=["sv20"], w=["rg_b"])
                    for s_, slot in enumerate(slots):
                        sl_ = slice(s_ * Lseg, (s_ + 1) * Lseg)
                        c0_ = s_ * Lseg
                        stt(rg_b[:, c0_:c0_ + 1], rg_a[:, c0_:c0_ + 1], hst[:, slot, gi:gi + 1], rg_b[:, c0_:c0_ + 1],
                            ALU.mult, ALU.add, r=["rg_a", "rg_b", "hst"], w=["rg_b"])
                        S.op("dve", lambda e, sl_=sl_: e.tensor_tensor_scan(
                            out=rg_h[:, sl_], data0=rg_a[:, sl_], data1=rg_b[:, sl_],
                            initial=0.0, op0=ALU.mult, op1=ALU.add),
                            ["rg_a", "rg_b"], ["rg_h"])
                        cp(hst[:, slot, gi:gi + 1], rg_h[:, (s_ + 1) * Lseg - 1:(s_ + 1) * Lseg], r=["rg_h"], w=["hst"])
                    tt(Dm[:, 16 + gi, 0:ntok], rg_t[:, 0:ntok], rg_h[:, 0:ntok], ALU.mult, r=["rg_t", "rg_h"], w=[Dk(16 + gi)])
                    yield
                if gp == 7:
                    if kind == "s":
                        dma("sp", oh, hst[:, 1:3, :], r=["hst"])
                        dma("sp", oconv, cvst[:, 1:3], r=["cvst"])
                    elif last_prompt:
                        dma("sp", ph, hst[:, 0, :], r=["hst"])
                        dma("sp", pconv, cvst[:, 0], r=["cvst"])

            def do_pair(gp):
                pb_ = gp % 2
                drain("b", 1)

                def xr_cons(j, bk, pb_=pb_):
                    act(xrh[:, pb_, j, 0:nseg * (Lseg + 3)].rearrange("p (s l) -> p s l", s=nseg)[:, :, 3:3 + Lseg],
                        PS[bk][:, 0:ntok].rearrange("p (s l) -> p s l", s=nseg), AF.Copy, r=[pk(bk)], w=[("xrh", pb_, j)])
                ws_group(w_in, 6152 + gp * 256, 2, 32, u_rhs, u_keys, ntok, xr_cons)

                def gr_cons(j, bk, pb_=pb_):
                    cp(grb[:, pb_, j, 0:ntok], PS[bk][:, 0:ntok], r=[pk(bk)], w=[("grb", pb_, j)])
                ws_group(w_in, 8200 + gp * 256, 2, 32, u_rhs, u_keys, ntok, gr_cons)
                lanes["b"].append(rg_gen(gp, pb_))

            for h in range(4):
                do_head(h)
                do_pair(2 * h)
                do_pair(2 * h + 1)
            drain()

            stage(7)
            d_rhs = lambda k: Dm[:, k, 0:ntok]
            d_keys = lambda k: [Dk(k)]
            for og in range(16):
                def mo_cons(j, bk, og=og):
                    c = og * 2 + j
                    if os.environ.get("MK_VAR") != "1":
                        cp(B[:, c, 0:ntok], PS[bk][:, 0:ntok], r=[pk(bk)], w=[Bk(c)])
                    if os.environ.get("MK_VAR") != "2":
                        accum_sq(c, PS[bk][:, 0:ntok], [pk(bk)], ntok)
                ws_group(w_out, og * 256, 2, 32, d_rhs, d_keys, ntok, mo_cons)
                stage(7.1)
            stage(7.2)
            finish_rstd(ntok, D)
            stage(7.3)
            for c in range(NCH):
                stt(B[:, c, 0:ntok], B[:, c, 0:ntok], gpm[:, c:c + 1], rstd[:, 0:ntok], ALU.mult, ALU.mult,
                    r=[Bk(c), "rstd", "vecs"], w=[Bk(c)])
                tt(A[:, c, 0:ntok], A[:, c, 0:ntok], B[:, c, 0:ntok], ALU.add, r=[Ak(c), Bk(c)], w=[Ak(c)])
            for c in range(NCH):
                accum_sq(c, A[:, c, 0:ntok], [Ak(c)], ntok)
            finish_rstd(ntok, D)
            for c in range(NCH):
                stt(U[:, c, 0:ntok], A[:, c, 0:ntok], gpf[:, c:c + 1], rstd[:, 0:ntok], ALU.mult, ALU.mult,
                    r=[Ak(c), "rstd", "vecs"], w=[Uk(c)])

            stage(8)
            parts = [(0, 15), (15, 14), (29, 14)]
            for pi, (g0, ng) in enumerate(parts):
                for gg in range(ng):
                    col = (g0 + gg) * 256
                    gb = []

                    def g_cons(j, bk):
                        gb.append(bk)
                    ws_group(w_g, col, 2, 32, u_rhs, u_keys, ntok, g_cons, hold=True)

                    def u_cons(j, bk, gg=gg):
                        gk = gb[j]
                        s = sqt[j]
                        act(s[:, 0:ntok], PS[gk][:, 0:ntok], AF.Silu, r=[pk(gk)], w=[("sqt", j)])
                        tt(Dm[:, gg * 2 + j, 0:ntok], s[:, 0:ntok], PS[bk][:, 0:ntok], ALU.mult,
                           r=[("sqt", j), pk(bk)], w=[Dk(gg * 2 + j)])
                        free(gk)
                    ws_group(w_u, col, 2, 32, u_rhs, u_keys, ntok, u_cons)
                K = ng * 2
                for og in range(8):
                    def dn_cons(j, bk, og=og, pi=pi):
                        c = og * 4 + j
                        if pi == 0:
                            cp(B[:, c, 0:ntok], PS[bk][:, 0:ntok], r=[pk(bk)], w=[Bk(c)])
                        else:
                            tt(B[:, c, 0:ntok], B[:, c, 0:ntok], PS[bk][:, 0:ntok], ALU.add, r=[Bk(c), pk(bk)], w=[Bk(c)])
                        if pi == 2:
                            accum_sq(c, B[:, c, 0:ntok], [Bk(c)], ntok)
                    ws_group(w_d[g0 * 256:(g0 + ng) * 256, :], og * 512, 4, K, d_rhs, d_keys, ntok, dn_cons)
            finish_rstd(ntok, D)
            for c in range(NCH):
                stt(B[:, c, 0:ntok], B[:, c, 0:ntok], gpo[:, c:c + 1], rstd[:, 0:ntok], ALU.mult, ALU.mult,
                    r=[Bk(c), "rstd", "vecs"], w=[Bk(c)])
                tt(B[:, c, 0:ntok], B[:, c, 0:ntok], A[:, c, 0:ntok], ALU.add, r=[Ak(c), Bk(c)], w=[Bk(c)])
            stage(9)
            for bi, (b0, L) in enumerate(blocks):
                for pc in range(8):
                    st = stg[pc % 2]; sk = ("stg", pc % 2)
                    bk = bank()
                    for j in range(4):
                        c = pc * 4 + j
                        tr(PS[bk][0:L, j * 128:(j + 1) * 128], B[:, c, b0:b0 + L], ident, r=[Bk(c), "cf"],
                           w=[pk(bk)], signal=(j == 3))
                    evac_copy(st[0:L, :], PS[bk][0:L, :], r=[pk(bk)], w=[sk])
                    free(bk)
                    dma("sp", yd[row0 + b0:row0 + b0 + L, pc * 512:(pc + 1) * 512], st[0:L, :], r=[sk])

        try:
            do_tile(n_ptiles, "s")
            for ti in range(n_ptiles):
                do_tile(ti, "p")
        except StopBuild:
            pass
        S.finish()

        sems = {sk: nc.alloc_semaphore(name=f"s_{sk[0]}_{sk[1]}") for sk in sorted(S.semkeys, key=str)}
        engs = {"pe": "tensor", "act": "scalar", "dve": "vector", "pool": "gpsimd", "sp": "sync"}

        def replay(e, name):
            for waits, fn, sig_ in S.ops[name]:
                for sk, v in waits:
                    e.wait_ge(sems[sk], v)
                if fn is None:
                    continue
                ins = fn(e)
                if sig_ is not None:
                    ins.then_inc(sems[sig_[0]], sig_[1])

        with nc.Block() as block:
            @block.tensor
            def _(e):
                replay(e, "pe")

            @block.scalar
            def _(e):
                replay(e, "act")

            @block.vector
            def _(e):
                replay(e, "dve")

            @block.gpsimd
            def _(e):
                replay(e, "pool")

            @block.sync
            def _(e):
                replay(e, "sp")
    return nc


def _consts():
    idx = np.arange(128)
    ident = np.eye(128, dtype=np.float32)
    tri = (idx[:, None] <= idx[None, :]).astype(np.float32)
    maskA = np.where(idx[None, :] <= idx[:, None], 0.0, -BIG).astype(np.float32)
    maskB = np.where(idx[:, None] <= idx[None, :], 0.0, BIG).astype(np.float32)
    ones = np.ones((128, 128), np.float32)
    cst = np.zeros((128, 8), np.float32)
    cst[:, 0] = EPS
    cst[:, 1] = 1.0
    cf = np.concatenate([ident, tri, maskA, maskB, ones, cst], axis=1)
    cb = np.concatenate([ident, ones], axis=1).astype(ml_dtypes.bfloat16)
    return np.ascontiguousarray(cf), np.ascontiguousarray(cb)


def _fm(v, nchunks):
    return np.ascontiguousarray(np.asarray(v, np.float32).reshape(nchunks, 128).T)


_NC_CACHE = {}


def _run(inputs, n_cores, n_ptiles):
    f = lambda k: np.asarray(inputs[k], np.float32)
    xp_all = f("x_prompt"); xs_all = f("x_sample")
    SEQ = n_ptiles * NT
    assert xp_all.shape[1] == SEQ
    if n_ptiles not in _NC_CACHE:
        _NC_CACHE[n_ptiles] = build_nc(n_ptiles)
    nc = _NC_CACHE[n_ptiles]
    cf, cb = _consts()
    vecs = np.zeros((128, 352), np.float32)
    vecs[:, 0:32] = _fm(f("g_pre_mix")[0], 32); vecs[:, 32:64] = _fm(f("g_post_mix")[0], 32)
    vecs[:, 64:96] = _fm(f("g_pre_ffn")[0], 32); vecs[:, 96:128] = _fm(f("g_post_ffn")[0], 32)
    vecs[:, 128:144] = _fm(f("g_mlstm_head")[0], 16)
    cwv = f("conv_w")[0]
    vecs[:, 144:208] = cwv.reshape(4, 16, 128).transpose(2, 1, 0).reshape(128, 64)
    vecs[:, 208:224] = _fm(f("conv_b")[0], 16); vecs[:, 224:240] = _fm(f("b_rg_a")[0], 16)
    vecs[:, 240:256] = _fm(f("b_rg_x")[0], 16); vecs[:, 256:272] = _fm(f("rg_lambda")[0], 16)
    bif = np.ascontiguousarray(np.broadcast_to(
        np.concatenate([f("b_igate")[0], f("b_fgate")[0]])[None, :], (128, 8)))
    shared = {
        "w_in": f("w_in")[0], "w_out": f("w_out")[0], "w_g": f("w_ffn_gate")[0], "w_u": f("w_ffn_up")[0],
        "w_d": f("w_ffn_down")[0], "w_rga": f("w_rg_a")[0], "w_rgx": f("w_rg_x")[0],
        "vecs": vecs, "bif": bif, "cf": cf, "cb": cb,
    }
    sC = f("state_mlstm_C")[0]; sn = f("state_mlstm_n")[0]; sm = f("state_mlstm_m")[0]
    sh = f("state_rglru_h")[0]; scv = f("state_rglru_conv")[0]
    in_maps = []
    for c in range(n_cores):
        sl = slice(2 * c, 2 * c + 2)
        m = dict(shared)
        m["xp"] = np.ascontiguousarray(xp_all[c])
        m["xs"] = np.ascontiguousarray(xs_all[sl].reshape(64, D))
        m["sC"] = np.ascontiguousarray(sC[sl])
        m["sn"] = np.ascontiguousarray(sn[sl].reshape(2, 4, 2, 128).transpose(3, 0, 1, 2))
        m["sm"] = np.ascontiguousarray(np.broadcast_to(sm[sl][None], (128, 2, 4)))
        m["sh"] = np.ascontiguousarray(sh[sl].reshape(2, 16, 128).transpose(2, 0, 1))
        m["sconv"] = np.ascontiguousarray(scv[sl].reshape(2, 3, 16, 128).transpose(3, 0, 2, 1))
        in_maps.append(m)
    res = run_bass_kernel_spmd(nc, in_maps, core_ids=list(range(n_cores)))
    R = res.results
    B_, DB = n_cores, 2 * n_cores
    y_p = np.stack([R[c]["yp"] for c in range(B_)])
    y_s = np.concatenate([R[c]["ys"].reshape(2, 32, D) for c in range(B_)])
    p_C = np.stack([R[c]["pC"] for c in range(B_)])[None]
    p_n = np.stack([R[c]["pn"].transpose(1, 2, 0).reshape(4, 256) for c in range(B_)])[None]
    p_m = np.stack([R[c]["pm"][0] for c in range(B_)])[None]
    p_h = np.stack([R[c]["ph"].T.reshape(2048) for c in range(B_)])[None]
    p_conv = np.stack([R[c]["pconv"].transpose(2, 1, 0).reshape(3, 2048) for c in range(B_)])[None]
    s_C = np.concatenate([R[c]["oC"] for c in range(B_)])[None]
    s_n = np.concatenate([R[c]["on"].transpose(1, 2, 3, 0).reshape(2, 4, 256) for c in range(B_)])[None]
    s_m = np.concatenate([R[c]["om"][0] for c in range(B_)])[None]
    s_h = np.concatenate([R[c]["oh"].transpose(1, 2, 0).reshape(2, 2048) for c in range(B_)])[None]
    s_conv = np.concatenate([R[c]["oconv"].transpose(1, 3, 2, 0).reshape(2, 3, 2048) for c in range(B_)])[None]
    outs = (y_p, y_s, p_C, p_n, p_m, p_h, p_conv, s_C, s_n, s_m, s_h, s_conv)
    return tuple(np.ascontiguousarray(o, dtype=np.float32) for o in outs)


def kernel(**inputs):
    return _run(inputs, 8, 8)
```
